# Optimizing a Trainium2 kernel written in Bass

```python
import math
import jax, jax.numpy as jnp
from jax import lax
import numpy as np

D_MODEL = 1024
BATCH = 2
SEQ = 8192
DEPTH = 1

N_Q_HEADS = 8
N_KV_HEADS = 2
HEAD_DIM = 64
ATTN_WIDTH = N_Q_HEADS * HEAD_DIM
KV_WIDTH = N_KV_HEADS * HEAD_DIM
WINDOW = 128
BLOCK = 128
POOL_WINDOWS = (2, 4, 8, 16)
N_POOL_GROUPS = len(POOL_WINDOWS)
POOL_GROUP_WIDTH = 128
POOL_WIDTH = N_POOL_GROUPS * POOL_GROUP_WIDTH
N_BRANCHES = 2
IN_WIDTH = ATTN_WIDTH + 2 * KV_WIDTH + POOL_WIDTH + N_BRANCHES * D_MODEL
N_EXPERTS = 32
TOP_K = 4
D_EXPERT = D_MODEL
SWIGLU_LIMIT = 7.0
SWIGLU_ALPHA = 1.702
MOE_BLOCK = 128
LN_EPS = 1e-5
DEEPNORM_ALPHA = (2.0 * DEPTH) ** 0.25
DEEPNORM_BETA = (8.0 * DEPTH) ** -0.25

kernel_name = 'hybrid_swa_pool_moe_deepnorm_encoder'


def layer_norm(x, g, b):
    xf = x.astype(jnp.float32)
    mean = jnp.mean(xf, axis=-1, keepdims=True)
    var = jnp.mean(jnp.square(xf - mean), axis=-1, keepdims=True)
    y = (xf - mean) * lax.rsqrt(var + LN_EPS)
    return y.astype(x.dtype) * g + b


def alibi_slopes(n_heads):
    return np.asarray([2.0 ** (-8.0 * (h + 1) / n_heads) for h in range(n_heads)], dtype=np.float32)


def windowed_attention(q, k, v, sinks):
    B, S = q.shape[0], q.shape[1]
    nb = S // BLOCK
    G = N_Q_HEADS // N_KV_HEADS
    qb = q.reshape(B, nb, BLOCK, N_KV_HEADS, G, HEAD_DIM)
    pad = ((0, 0), (BLOCK, BLOCK), (0, 0), (0, 0))
    kp = jnp.pad(k, pad).reshape(B, nb + 2, BLOCK, N_KV_HEADS, HEAD_DIM)
    vp = jnp.pad(v, pad).reshape(B, nb + 2, BLOCK, N_KV_HEADS, HEAD_DIM)
    kw = jnp.concatenate([kp[:, :-2], kp[:, 1:-1], kp[:, 2:]], axis=2)
    vw = jnp.concatenate([vp[:, :-2], vp[:, 1:-1], vp[:, 2:]], axis=2)
    scores = jnp.einsum('bnqhgd,bnkhd->bnhgqk', qb, kw).astype(jnp.float32)
    scores = scores * (1.0 / math.sqrt(HEAD_DIM))
    rel = np.arange(3 * BLOCK)[None, :] - BLOCK - np.arange(BLOCK)[:, None]
    key_abs = np.arange(nb)[:, None] * BLOCK - BLOCK + np.arange(3 * BLOCK)[None, :]
    valid = (np.abs(rel) <= WINDOW)[None, :, :] & ((key_abs >= 0) & (key_abs < S))[:, None, :]
    slopes = jnp.asarray(alibi_slopes(N_Q_HEADS)).reshape(N_KV_HEADS, G)
    bias = -slopes[:, :, None, None] * jnp.asarray(np.abs(rel), jnp.float32)[None, None]
    scores = jnp.where(jnp.asarray(valid)[None, :, None, None], scores + bias[None, None], -jnp.inf)
    sink = sinks.astype(jnp.float32).reshape(N_KV_HEADS, G)[None, None, :, :, None, None]
    m = jnp.maximum(jnp.max(scores, axis=-1, keepdims=True), sink)
    p = jnp.exp(scores - m)
    probs = p / (jnp.sum(p, axis=-1, keepdims=True) + jnp.exp(sink - m))
    out = jnp.einsum('bnhgqk,bnkhd->bnqhgd', probs.astype(v.dtype), vw)
    return out.reshape(B, S, ATTN_WIDTH)


def multiscale_pool(p, w_group, scale):
    B, S = p.shape[0], p.shape[1]
    cs = jnp.concatenate([jnp.zeros((B, 1, POOL_WIDTH), jnp.float32),
                          jnp.cumsum(p.astype(jnp.float32), axis=1)], axis=1)
    t = np.arange(S)
    outs = []
    for g, w in enumerate(POOL_WINDOWS):
        sl = slice(g * POOL_GROUP_WIDTH, (g + 1) * POOL_GROUP_WIDTH)
        lo = np.maximum(t - w // 2, 0)
        hi = np.minimum(t + w // 2 - 1, S - 1)
        cnt = jnp.asarray((hi - lo + 1).astype(np.float32))[None, :, None]
        csg = cs[:, :, sl]
        pooled = (csg[:, hi + 1] - csg[:, lo]) / cnt
        outs.append(pooled.astype(p.dtype) - p[:, :, sl])
    y = jnp.stack(outs, axis=2)
    y = jnp.einsum('bsgc,gcd->bsgd', y, w_group).reshape(B, S, POOL_WIDTH)
    return y * scale


def moe_ffn(x, w_router, b_router, w_mlp1, b_mlp1, w_mlp2, b_mlp2):
    B, S, D = x.shape
    N = B * S
    A = N * TOP_K
    xt = x.reshape(N, D)
    logits = (xt @ w_router + b_router).astype(jnp.float32)
    top_vals, top_idx = lax.top_k(logits, TOP_K)
    gates = jax.nn.softmax(top_vals, axis=-1)
    e_flat = top_idx.reshape(A)
    tok_flat = jnp.repeat(jnp.arange(N, dtype=jnp.int32), TOP_K)
    w_flat = gates.reshape(A)
    order = jnp.argsort(e_flat)
    e_sorted = e_flat[order]
    counts = jnp.bincount(e_flat, length=N_EXPERTS)
    padded = ((counts + MOE_BLOCK - 1) // MOE_BLOCK) * MOE_BLOCK
    start = jnp.cumsum(counts) - counts
    pend = jnp.cumsum(padded)
    pstart = pend - padded
    dest = pstart[e_sorted] + (jnp.arange(A) - start[e_sorted])
    R = A + N_EXPERTS * MOE_BLOCK
    nblk = R // MOE_BLOCK
    row_tok = jnp.zeros((R,), jnp.int32).at[dest].set(tok_flat[order])
    row_w = jnp.zeros((R,), jnp.float32).at[dest].set(w_flat[order])
    blk_expert = jnp.minimum(
        jnp.searchsorted(pend, jnp.arange(nblk) * MOE_BLOCK, side='right'), N_EXPERTS - 1)
    xs = xt[row_tok].reshape(nblk, MOE_BLOCK, D)

    def expert_block(args):
        xb, e = args
        h = xb @ w_mlp1[e] + b_mlp1[e]
        gate = jnp.minimum(h[:, :D_EXPERT], SWIGLU_LIMIT)
        up = jnp.clip(h[:, D_EXPERT:], -SWIGLU_LIMIT, SWIGLU_LIMIT)
        act = (up + 1.0) * gate * jax.nn.sigmoid(SWIGLU_ALPHA * gate)
        return act @ w_mlp2[e] + b_mlp2[e]

    ys = lax.map(expert_block, (xs, blk_expert)).reshape(R, D)
    ys = ys * row_w[:, None].astype(ys.dtype)
    out = jax.ops.segment_sum(ys, row_tok, num_segments=N)
    return out.reshape(B, S, D)


def setup_inputs(seed: int = 0) -> dict:
    key = jax.random.key(seed)
    ks = jax.random.split(key, 20)
    L, D, E, F = DEPTH, D_MODEL, N_EXPERTS, D_EXPERT
    nrm = lambda k, shape: jax.random.normal(k, shape, jnp.float32)
    col_scale = np.ones((IN_WIDTH,), np.float32)
    v0 = ATTN_WIDTH + KV_WIDTH
    col_scale[v0:v0 + KV_WIDTH] = DEEPNORM_BETA
    w_in = nrm(ks[1], (L, D, IN_WIDTH)) * (D ** -0.5) * jnp.asarray(col_scale)
    return {
        'x': nrm(ks[0], (BATCH, SEQ, D)),
        'w_in': w_in,
        'attn_sinks': nrm(ks[2], (L, N_Q_HEADS)),
        'w_attn_branch': nrm(ks[3], (L, ATTN_WIDTH, D)) * (ATTN_WIDTH ** -0.5),
        'w_pool_group': nrm(ks[4], (L, N_POOL_GROUPS, POOL_GROUP_WIDTH, POOL_GROUP_WIDTH)) * (POOL_GROUP_WIDTH ** -0.5),
        'pool_scale': 1.0 + 0.1 * nrm(ks[5], (L, POOL_WIDTH)),
        'w_pool_branch': nrm(ks[6], (L, POOL_WIDTH, D)) * (POOL_WIDTH ** -0.5),
        'w_out': nrm(ks[7], (L, D, D)) * (D ** -0.5) * DEEPNORM_BETA,
        'ln1_g': 1.0 + 0.02 * nrm(ks[8], (L, D)),
        'ln1_b': 0.02 * nrm(ks[9], (L, D)),
        'w_router': nrm(ks[10], (L, D, E)) * (D ** -0.5),
        'b_router': 0.01 * nrm(ks[11], (L, E)),
        'w_mlp1': nrm(ks[12], (L, E, D, 2 * F)) * (D ** -0.5) * DEEPNORM_BETA,
        'b_mlp1': 0.01 * nrm(ks[13], (L, E, 2 * F)),
        'w_mlp2': nrm(ks[14], (L, E, F, D)) * (F ** -0.5) * DEEPNORM_BETA,
        'b_mlp2': 0.01 * nrm(ks[15], (L, E, D)),
        'ln2_g': 1.0 + 0.02 * nrm(ks[16], (L, D)),
        'ln2_b': 0.02 * nrm(ks[17], (L, D)),
    }


def reference(x, w_in, attn_sinks, w_attn_branch, w_pool_group, pool_scale, w_pool_branch,
              w_out, ln1_g, ln1_b, w_router, b_router, w_mlp1, b_mlp1, w_mlp2, b_mlp2,
              ln2_g, ln2_b):
    B, S, D = x.shape
    o_k = ATTN_WIDTH
    o_v = o_k + KV_WIDTH
    o_p = o_v + KV_WIDTH
    o_g = o_p + POOL_WIDTH
    h = x
    for l in range(DEPTH):
        u = h @ w_in[l]
        q = u[..., :o_k].reshape(B, S, N_Q_HEADS, HEAD_DIM)
        k = u[..., o_k:o_v].reshape(B, S, N_KV_HEADS, HEAD_DIM)
        v = u[..., o_v:o_p].reshape(B, S, N_KV_HEADS, HEAD_DIM)
        p_in = u[..., o_p:o_g]
        g_attn = jax.nn.sigmoid(u[..., o_g:o_g + D])
        g_pool = jax.nn.sigmoid(u[..., o_g + D:o_g + 2 * D])
        a_br = windowed_attention(q, k, v, attn_sinks[l]) @ w_attn_branch[l]
        p_br = multiscale_pool(p_in, w_pool_group[l], pool_scale[l]) @ w_pool_branch[l]
        mixed = g_attn * a_br + g_pool * p_br
        h = layer_norm(DEEPNORM_ALPHA * h + mixed @ w_out[l], ln1_g[l], ln1_b[l])
        f = moe_ffn(h, w_router[l], b_router[l], w_mlp1[l], b_mlp1[l], w_mlp2[l], b_mlp2[l])
        h = layer_norm(DEEPNORM_ALPHA * h + f, ln2_g[l], ln2_b[l])
    return h
```

```python
import numpy as np
import concourse.bass as bass
import concourse.mybir as mybir
from concourse.bass_utils import run_bass_kernel_spmd

F32 = mybir.dt.float32
F32R = mybir.dt.float32r
I32 = mybir.dt.int32
AF = mybir.ActivationFunctionType
ALU = mybir.AluOpType
AX = mybir.AxisListType

NCORES = 8
D = 1024
SEQ = 8192
TOK = 2048
TT = 512
NTILE = 4
SLAB = 768
NE = 32
CAP = 384
PW0 = 112
PWN = 544
ALPHA = 2.0 ** 0.25
LN_EPS = 1e-5
RSLOT = 4096
NRING = 5


class Sched:
    def __init__(self, sems):
        self.pool_sems = list(sems)
        self.eng = {}
        for n in ("pe", "act", "dve", "gp", "sp"):
            self.eng[n] = dict(sem=self.pool_sems.pop(), count=0, prog=[], seen={}, pend={})
        self.dsem = {}
        self.buf = {}
        self.fence = {}

    def _b(self, k):
        if k not in self.buf:
            self.buf[k] = dict(w={}, r={})
        return self.buf[k]

    @staticmethod
    def _mx(d, tok):
        s, v = tok
        key = id(s)
        if key not in d or d[key][1] < v:
            d[key] = (s, v)

    def op(self, eng, fn, reads=(), writes=(), cwrites=(), dma=None):
        E = self.eng[eng]
        fr, fw = set(), set()
        for k in list(reads) + list(writes) + list(cwrites):
            if k in self.fence:
                fk, mode = self.fence[k]
                (fr if mode == "X" else fw).add(fk)
        if fr or fw:
            reads = list(reads) + sorted(fr)
            cwrites = list(cwrites) + sorted(fw)
        deps = {}
        raw = {}
        for k in reads:
            for t in self._b(k)["w"].values():
                self._mx(deps, t)
                self._mx(raw, t)
        for k in writes:
            b = self._b(k)
            for t in b["w"].values():
                self._mx(deps, t)
            for t in b["r"].values():
                self._mx(deps, t)
        for k in cwrites:
            for t in self._b(k)["r"].values():
                self._mx(deps, t)
        for t in E["pend"].values():
            self._mx(deps, t)
        E["pend"] = {}
        waits = []
        for key, (s, v) in deps.items():
            if s is E["sem"]:
                if eng == "pe" or key not in raw:
                    continue
                v = raw[key][1]
            if E["seen"].get(key, 0) >= v:
                continue
            E["seen"][key] = v
            waits.append((s, v))
        if dma is None:
            E["count"] += 1
            tok = (E["sem"], E["count"])
            inc = (E["sem"], 1)
        else:
            if dma not in self.dsem:
                self.dsem[dma] = [self.pool_sems.pop(), 0]
            ds = self.dsem[dma]
            ds[1] += 16
            tok = (ds[0], ds[1])
            inc = (ds[0], 16)
        E["prog"].append((waits, fn, inc))
        for k in reads:
            self._mx(self._b(k)["r"], tok)
        for k in writes:
            b = self._b(k)
            b["w"] = {}
            b["r"] = {}
            self._mx(b["w"], tok)
        for k in cwrites:
            self._mx(self._b(k)["w"], tok)
        return tok

    def barrier(self):
        toks = [(e["sem"], e["count"]) for e in self.eng.values() if e["count"] > 0]
        toks += [(d[0], d[1]) for d in self.dsem.values() if d[1] > 0]
        for e in self.eng.values():
            for t in toks:
                if t[0] is not e["sem"]:
                    self._mx(e["pend"], t)

    def final_waits(self, eng):
        E = self.eng[eng]
        toks = [(e["sem"], e["count"]) for n, e in self.eng.items() if e["count"] > 0 and n != eng]
        toks += [(d[0], d[1]) for d in self.dsem.values() if d[1] > 0]
        E["final"] = toks

    def replay(self, eng, h):
        E = self.eng[eng]
        for waits, fn, (s, n) in E["prog"]:
            for (ws, wv) in waits:
                h.wait_ge(ws, wv)
            inst = fn(h)
            inst.then_inc(s, n)
        for (ws, wv) in E.get("final", []):
            h.wait_ge(ws, wv)


def I(m, *a, **k):
    return lambda h: getattr(h, m)(*a, **k)


def G(calls):
    def f(h):
        r = None
        for (m, a, k) in calls:
            r = getattr(h, m)(*a, **k)
        return r
    return f


def build_program(stage="full"):
    nc = bass.Bass("TRN2", target_bir_lowering=False)
    full = stage == "full"
    STOP = int(stage[1:2]) if stage.startswith("s") else 99
    SUB = stage[2:] if stage.startswith("s") else "z"

    def din(name, shape, dt=F32):
        return nc.dram_tensor(name, list(shape), dt, kind="ExternalInput").ap()

    xs = din("xs", [NTILE, SLAB, D])
    kval_d = din("kval", [NTILE, 128, 6])
    icnt_d = din("icnt", [NTILE, 128, 64])
    ebias_d = din("ebias", [128, 3072])
    ident_d = din("ident", [128, 128])
    tri_d = din("tri", [128, 128])
    ones_d = din("ones", [128, 128])
    ecol_d = din("ecol", [128, NE])
    sinks_d = din("sinks", [128, 8])
    ln1g_d = din("ln1g", [128, D])
    ln1b_d = din("ln1b", [128, D])
    pscale_d = din("pscale", [128, 4])
    brout_d = din("brout", [128, NE])
    wr_d = din("wr", [D, NE])
    w_in = din("w_in", [D, 3328])
    wab = din("wab", [512, D])
    wpg = din("wpg", [512, 128])
    wpb = din("wpb", [512, D])
    wout = din("wout", [D, D])
    if full:
        ln2g_d = din("ln2g", [128, D])
        ln2b_d = din("ln2b", [128, D])
        b1T_d = din("b1T", [128, NE * 16])
        b2_d = din("b2", [NE, D])
        w1 = din("w1", [NE, D, 2 * D])
        w2 = din("w2", [NE, D, D])
        out_d = nc.dram_tensor("out", [TOK, D], F32, kind="ExternalOutput").ap()
    xs_scr = nc.dram_tensor("xs_scr", [NE * CAP + 128, D], F32, kind="Internal").ap()
    ys_scr = nc.dram_tensor("ys_scr", [NE * CAP + 128, D], F32, kind="Internal").ap()
    h1_scr = nc.dram_tensor("h1_scr", [TOK, D], F32, kind="Internal" if full else "ExternalOutput").ap()
    if not full:
        dbg_attnT = nc.dram_tensor("dbg_attnT", [128, 2048], F32, kind="ExternalOutput").ap()
        dbg_zsT = nc.dram_tensor("dbg_zsT", [128, 2048], F32, kind="ExternalOutput").ap()
        dbg_mT = nc.dram_tensor("dbg_mT", [128, 4096], F32, kind="ExternalOutput").ap()
        dbg_qT = nc.dram_tensor("dbg_qT", [128, 2048], F32, kind="ExternalOutput").ap()
        dbg_kT = nc.dram_tensor("dbg_kT", [128, 1536], F32, kind="ExternalOutput").ap()
        dbg_v = nc.dram_tensor("dbg_v", [128, 792], F32, kind="ExternalOutput").ap()
        dbg_yT = nc.dram_tensor("dbg_yT", [128, 2048], F32, kind="ExternalOutput").ap()
        dbg_idx = nc.dram_tensor("dbg_idx", [128, 64], I32, kind="ExternalOutput").ap()
        dbg_gates = nc.dram_tensor("dbg_gates", [128, 64], F32, kind="ExternalOutput").ap()

    WNR = 17688
    WNF = 12992
    from contextlib import ExitStack
    with ExitStack() as es:
        def sb(name, shape, dt=F32):
            return es.enter_context(nc.sbuf_tensor(name, list(shape), dt))

        workr = sb("workr", [128, WNR])
        workf = sb("workf", [128, WNF])
        ring_t = sb("ring", [128, NRING * RSLOT], F32R)
        ident = sb("ident_s", [128, 128])
        tri = sb("tri_s", [128, 128])
        ones = sb("ones_s", [128, 128])
        ecol = sb("ecol_s", [128, NE])
        esink = sb("esink", [128, 8])
        pscale = sb("pscale_s", [128, 4])
        brout = sb("brout_s", [128, NE])
        b1T = sb("b1T_s", [128, NE * 16])
        wr = sb("wr_s", [128, 8 * NE])
        runcnt = sb("runcnt", [128, NE])
        gates_all = sb("gates_all", [128, 16 * 4])
        idx_all = sb("idx_all", [128, 16 * 4], I32)
        kval = sb("kval_s", [128, 6])
        icnt = sb("icnt_s", [128, 64])
        small = sb("small", [128, 256])
        psb = [es.enter_context(nc.psum_tensor(f"ps{i}", [128, 512], F32)) for i in range(8)]
        sems = [es.enter_context(nc.semaphore(f"s{i}")) for i in range(80)]
        S = Sched(sems)
        for k_ in ["qT", "kT", "vaug", "PT0", "PT1"] + [f"yT{g_}" for g_ in range(4)]:
            S.fence[k_] = ("FXr", "X")
        S.fence["mT"] = ("FXr", "Y")
        for k_ in ["expS0", "expS1", "attn_tok", "ptmp0", "ptmp1", "h1T"] + [f"pT{g_}" for g_ in range(4)]:
            S.fence[k_] = ("FXf", "X")
        for k_ in ["sa", "spb", "r1_0", "r1_1", "r1_2", "ntmp"]:
            S.fence[k_] = ("FXf", "Y")
        block = es.enter_context(nc.Block())

        def carve(work, off, shape):
            n = int(np.prod(shape[1:]))
            ap = work[:, off:off + n]
            if len(shape) == 3:
                ap = ap.rearrange("p (a b) -> p a b", a=shape[1])
            elif len(shape) == 4:
                ap = ap.rearrange("p (a b c) -> p a b c", a=shape[1], b=shape[2])
            return ap

        def R(ap):
            return ap.bitcast(F32R)

        def r3(ap, a):
            return ap.rearrange("p (a b) -> p a b", a=a)

        psn = [0]
        reserved = set()

        def bank():
            while True:
                i = psn[0] % 8
                psn[0] += 1
                if i not in reserved:
                    return psb[i], f"ps{i}", i

        NR2 = 7
        XOFF = 8320

        def ring(slot):
            if slot < NRING:
                return ring_t[:, slot * RSLOT:(slot + 1) * RSLOT]
            o_ = XOFF + (slot - NRING) * RSLOT
            return workr[:, o_:o_ + RSLOT].bitcast(F32R)

        pieces = []

        slot_of = {}
        next_on_slot = {}

        def emit_piece_load(n):
            slot = slot_of[n]
            sl = ring(slot)
            first = True
            for (dfn, src) in pieces[n]:
                dst = dfn(sl)
                kw = dict(writes=[f"ring{slot}"]) if first else dict(cwrites=[f"ring{slot}"])
                S.op("gp", I("dma_start", out=dst, in_=src), dma=f"ring{slot}", **kw)
                first = False

        def piece_done(n):
            m = next_on_slot.get(n)
            if m is not None:
                emit_piece_load(m)

        offs = {"r": 0, "f": 0}

        def take(shape, a):
            work, lim = (workr, WNR) if a == "r" else (workf, WNF)
            ap = carve(work, offs[a], shape)
            offs[a] += int(np.prod(shape[1:]))
            assert offs[a] <= lim, (a, offs[a])
            return ap

        def tR(shape):
            return take(shape, "r")

        def tF(shape):
            return take(shape, "f")
        xtok = tF([128, 2, D])
        ebias = tF([128, 3, 8, 128])
        ln1g = tF([128, D])
        ln1b = tF([128, D])
        xT = tR([128, 8, SLAB])
        attnT = tR([128, 4, TT])
        zsT = tR([128, 4, TT])
        o_r, o_f = offs["r"], offs["f"]
        qT = tR([128, 4, TT])
        kT0 = tR([128, SLAB])
        kT1 = tR([128, SLAB])
        vaug = tR([128, 6, 2, 66])
        PT2 = tR([128, 2, 512])
        yT = tR([128, 4, TT])
        expS2 = tF([128, 2, 512])
        attn_tok = tF([128, 512])
        pT = tF([128, 4, PWN])
        ptmp = tF([128, 2, PWN])
        h1T = tF([128, 8, 128])
        offs["r"], offs["f"] = o_r, o_f
        mT = tR([128, 8, TT])
        sa = tF([128, TT])
        spb = tF([128, TT])
        r1 = tF([128, 3, D])
        ntmp1 = tF([128, D])

        lg = small[:, 0:32]
        m8 = small[:, 32:40]
        mask = small[:, 40:72]
        posb = small[:, 72:104]
        dest = small[:, 104:136]
        ovf = small[:, 136:168]
        junk = small[:, 168:200]
        destf = small[:, 200:204]
        negm = small[:, 204:205]
        ex4 = small[:, 208:212]
        gs = small[:, 212:213]
        rs = small[:, 213:214]
        stats = small[:, 216:228]
        mv = small[:, 228:230]
        rstd = small[:, 230:231]
        nmr = small[:, 231:232]
        den = small[:, 232:240]
        rec = small[:, 240:248]
        eps_t = small[:, 248:249]

        w_in_v = w_in.rearrange("(kc p) n -> p kc n", p=128)
        wout_v = wout.rearrange("(kc p) n -> p kc n", p=128)
        wab_v = wab.rearrange("(kc p) n -> p kc n", p=128)
        wpb_v = wpb.rearrange("(kc p) n -> p kc n", p=128)
        wpg_v = wpg.rearrange("(g c) d -> c g d", c=128)
        wq_v = w_in[:, 0:512].rearrange("(kc p) (t i d) -> p kc i t d", p=128, t=2, i=4)

        def v3(a, b):
            return lambda sl, a=a, b=b: sl[:, 0:a * b].rearrange("p (a b) -> p a b", a=a)

        def v3o(o_, a, b):
            return lambda sl, o_=o_, a=a, b=b: sl[:, o_:o_ + a * b].rearrange("p (a b) -> p a b", a=a)

        def vq(i, t_):
            return lambda sl, i=i, t_=t_: sl[:, 0:4096].rearrange("p (kc i t d) -> p kc i t d", kc=8, i=4, t=2)[:, :, i, t_, :]

        PI = {}
        for t in range(NTILE):
            PI[(t, "kv")] = len(pieces); pieces.append([(v3(8, 256), w_in_v[:, :, 512:768])])
            PI[(t, "q")] = len(pieces)
            pieces.append([(vq(i, t_), wq_v[:, :, i, t_, :]) for i in range(4) for t_ in range(2)])
            PI[(t, "p")] = len(pieces); pieces.append([(v3(8, 512), w_in_v[:, :, 768:1280])])
            PI[(t, "pg")] = len(pieces); pieces.append([(v3(4, 128), wpg_v)])
            for h_ in range(2):
                PI[(t, "br", h_)] = len(pieces)
                pieces.append([(v3o(0, 4, 512), wab_v[:, :, h_ * 512:(h_ + 1) * 512]),
                               (v3o(2048, 4, 512), wpb_v[:, :, h_ * 512:(h_ + 1) * 512])])
                for jj in (2 * h_, 2 * h_ + 1):
                    PI[(t, "g", jj)] = len(pieces)
                    pieces.append([(v3o(0, 8, 256), w_in_v[:, :, 1280 + jj * 256:1280 + (jj + 1) * 256]),
                                   (v3o(2048, 8, 256), w_in_v[:, :, 2304 + jj * 256:2304 + (jj + 1) * 256])])
            for nh in range(2):
                PI[(t, "wo", nh)] = len(pieces)
                pieces.append([(v3(8, 512), wout_v[:, :, nh * 512:(nh + 1) * 512])])
        if full:
            for e in range(NE):
                w1e = w1[e].rearrange("(kc p) (t g c) -> p kc t g c", p=128, t=2, g=2)
                w2e = w2[e].rearrange("(fc p) n -> p fc n", p=128)
                for cg_ in range(2):
                    for kh in range(2):
                        PI[(e, "w1", cg_, kh)] = len(pieces)
                        pieces.append([(lambda sl, t_=t_: sl[:, 0:4096].rearrange("p (kc t c) -> p kc t c", kc=4, t=2)[:, :, t_, :],
                                        w1e[:, kh * 4:(kh + 1) * 4, t_, cg_, :]) for t_ in range(2)])
                for kk in range(2):
                    PI[(e, "w2", kk)] = len(pieces)
                    pieces.append([(v3(4, 1024), w2e[:, kk * 4:(kk + 1) * 4, :])])

        n0_moe = PI[(0, "w1", 0, 0)] if full else len(pieces)
        last = {}
        for n in range(len(pieces)):
            sl_ = n % NRING if n < n0_moe else (n - n0_moe) % NR2
            slot_of[n] = sl_
            if sl_ in last:
                next_on_slot[last[sl_]] = n
            last[sl_] = n

        def pslot(key):
            n = PI[key]
            return n, ring(slot_of[n]), f"ring{slot_of[n]}"

        def ld(dst, src, key, sem=None):
            S.op("sp", I("dma_start", out=dst, in_=src), writes=[key], dma=(sem or "c_" + key))

        ld(ident[:], ident_d, "ident")
        ld(tri[:], tri_d, "tri")
        ld(ones[:], ones_d, "ones")
        ld(ecol[:], ecol_d, "ecol")
        ld(esink[:], sinks_d, "esink")
        ld(pscale[:], pscale_d, "pscale")
        ld(brout[:], brout_d, "brout")
        if full:
            ld(b1T[:], b1T_d, "b1T")
        ld(r3(wr[:], 8), wr_d.rearrange("(kc p) e -> p kc e", p=128), "wr")
        ld(ebias.rearrange("p a b c -> p (a b c)"), ebias_d, "ebias")
        ld(ln1g, ln1g_d, "ln1g")
        ld(ln1b, ln1b_d, "ln1b")
        for n in range(min(NRING, len(pieces))):
            emit_piece_load(n)
        S.op("act", I("activation", out=esink[:], in_=esink[:], func=AF.Exp), reads=["esink"], writes=["esink"])
        S.op("dve", I("memset", runcnt[:], 0.0), writes=["runcnt"])
        S.op("dve", I("memset", eps_t, LN_EPS), writes=["eps"])
        zf = ptmp.rearrange("p a b -> p (a b)")

        def zero_fill():
            S.op("dve", I("memset", zf, 0.0), writes=["ptmp0", "ptmp1"])
            S.op("dve", I("tensor_copy", R(kT0[64:128, :]), zf[64:128, 0:SLAB]), reads=["ptmp0", "ptmp1"], cwrites=["kT"])
            S.op("dve", I("tensor_copy", R(kT1[0:64, :]), zf[0:64, 0:SLAB]), reads=["ptmp0", "ptmp1"], cwrites=["kT"])
            for hd in range(2):
                S.op("dve", I("tensor_copy", R(vaug[:, :, hd, 65]), zf[:, 0:6]), reads=["ptmp0", "ptmp1"], cwrites=["vaug"])

        alt = [0]

        def evac(out_ap, in_ap, reads, writes=(), cwrites=(), eng=None):
            if eng is None:
                eng = "act" if alt[0] % 2 == 0 else "dve"
                alt[0] += 1
            if eng == "act":
                S.op("act", I("copy", out=out_ap, in_=in_ap), reads=reads, writes=writes, cwrites=cwrites)
            else:
                S.op("dve", I("tensor_copy", out_ap, in_ap), reads=reads, writes=writes, cwrites=cwrites)

        def layer_norm_tok(src, gam, bet, dst, ntmp, key_src, key_dst, gkey, bkey, nkey="ntmp"):
            for c in range(2):
                S.op("dve", I("bn_stats", out=stats[:, c * 6:(c + 1) * 6], in_=src[:, c * 512:(c + 1) * 512]),
                     reads=[key_src], cwrites=["stats"])
            S.op("dve", I("bn_aggr", out=mv, in_=stats), reads=["stats"], writes=["mv"])
            S.op("act", I("activation", out=rstd, in_=mv[:, 1:2], func=AF.Ln, bias=eps_t, scale=1.0),
                 reads=["mv", "eps"], writes=["rstd"])
            S.op("act", I("activation", out=rstd, in_=rstd, func=AF.Exp, scale=-0.5), reads=["rstd"], writes=["rstd"])
            S.op("dve", I("tensor_scalar", out=nmr, in0=mv[:, 0:1], scalar1=rstd, scalar2=-1.0,
                          op0=ALU.mult, op1=ALU.mult), reads=["mv", "rstd"], writes=["nmr", "stats"])
            S.op("act", I("activation", out=ntmp, in_=src, func=AF.Identity, bias=nmr, scale=rstd),
                 reads=[key_src, "rstd", "nmr"], writes=[nkey])
            S.op("dve", I("tensor_tensor", out=ntmp, in0=ntmp, in1=gam, op=ALU.mult),
                 reads=[nkey, gkey], writes=[nkey])
            S.op("dve", I("tensor_tensor", out=dst, in0=ntmp, in1=bet, op=ALU.add),
                 reads=[nkey, bkey], writes=[key_dst])

        def transp4(dst_bank, src_aps):
            return G([("transpose", (), dict(out=dst_bank[:, c * 128:(c + 1) * 128], in_=a, identity=ident[:]))
                      for c, a in enumerate(src_aps)])

        def mmgroup(out_ap, pairs):
            n = len(pairs)
            return G([("matmul", (out_ap, l, r), dict(start=(i == 0), stop=(i == n - 1)))
                      for i, (l, r) in enumerate(pairs)])

        wrv = r3(wr[:], 8)

        def router(t):
            for tb in range(4):
                tbg = t * 4 + tb
                rb = xtok[:, tb % 2, :]
                rk = f"xtok{tb % 2}"
                S.op("sp", I("dma_start", out=rb, in_=h1_scr[tbg * 128:(tbg + 1) * 128, :]),
                     reads=["h1_scr"], writes=[rk], dma=rk)
                for hb in range(2):
                    pb_, pk, _ = bank()
                    S.op("pe", transp4(pb_, [rb[:, (hb * 4 + c) * 128:(hb * 4 + c + 1) * 128] for c in range(4)]),
                         reads=[rk, "ident"], writes=[pk])
                    evac(h1T[:, hb * 4:(hb + 1) * 4, :], r3(pb_[:, :], 4), reads=[pk], cwrites=["h1T"])
                pb_, pk, _ = bank()
                S.op("pe", mmgroup(pb_[:, 0:32], [(h1T[:, kc, :], wrv[:, kc, :]) for kc in range(8)]),
                     reads=["h1T", "wr"], writes=[pk])
                S.op("dve", I("tensor_tensor", out=lg, in0=pb_[:, 0:32], in1=brout[:], op=ALU.add),
                     reads=[pk, "brout"], writes=["lg"])
                S.op("dve", I("max", out=m8, in_=lg), reads=["lg"], writes=["m8"])
                S.op("dve", I("tensor_scalar", out=mask, in0=lg, scalar1=m8[:, 3:4], scalar2=None, op0=ALU.is_ge),
                     reads=["lg", "m8"], writes=["mask"])
                pb2, pk2, _ = bank()
                S.op("pe", I("matmul", pb2[:, 0:32], tri[:], mask, start=True, stop=True), reads=["tri", "mask"], writes=[pk2])
                S.op("pe", I("matmul", pb2[:, 32:64], ones[:], mask, start=True, stop=True), reads=["ones", "mask"], cwrites=[pk2])
                S.op("dve", I("tensor_tensor", out=posb, in0=pb2[:, 0:32], in1=runcnt[:], op=ALU.add),
                     reads=[pk2, "runcnt"], writes=["posb"])
                S.op("dve", I("tensor_tensor", out=runcnt[:], in0=runcnt[:], in1=pb2[:, 32:64], op=ALU.add),
                     reads=[pk2, "runcnt"], writes=["runcnt"])
                S.op("dve", I("tensor_scalar", out=ovf, in0=posb, scalar1=float(CAP), scalar2=None, op0=ALU.is_ge),
                     reads=["posb"], writes=["ovf"])
                S.op("dve", I("tensor_tensor", out=dest, in0=posb, in1=ecol[:], op=ALU.add),
                     reads=["posb", "ecol"], writes=["dest"])
                S.op("dve", I("tensor_scalar", out=posb, in0=dest, scalar1=-1.0, scalar2=float(NE * CAP),
                              op0=ALU.mult, op1=ALU.add), reads=["dest"], writes=["posb"])
                S.op("dve", I("tensor_tensor", out=posb, in0=posb, in1=ovf, op=ALU.mult), reads=["posb", "ovf"], writes=["posb"])
                S.op("dve", I("tensor_tensor", out=dest, in0=dest, in1=posb, op=ALU.add), reads=["dest", "posb"], writes=["dest"])
                for k in range(4):
                    S.op("dve", I("scalar_tensor_tensor", out=junk, in0=lg, scalar=m8[:, k:k + 1], in1=dest,
                                  op0=ALU.is_equal, op1=ALU.mult, accum_out=destf[:, k:k + 1]),
                         reads=["lg", "m8", "dest"], writes=["junk"], cwrites=["destf"])
                S.op("dve", I("tensor_copy", idx_all[:, tbg * 4:(tbg + 1) * 4], destf), reads=["destf"], writes=[f"idx{tbg}"])
                S.op("dve", I("tensor_scalar", out=negm, in0=m8[:, 0:1], scalar1=-1.0, scalar2=None, op0=ALU.mult),
                     reads=["m8"], writes=["negm"])
                S.op("act", I("activation", out=ex4, in_=m8[:, 0:4], func=AF.Exp, bias=negm, scale=1.0, accum_out=gs),
                     reads=["m8", "negm"], writes=["ex4", "gs"])
                S.op("dve", I("reciprocal", out=rs, in_=gs), reads=["gs"], writes=["rs"])
                S.op("dve", I("tensor_scalar", out=gates_all[:, tbg * 4:(tbg + 1) * 4], in0=ex4, scalar1=rs,
                              scalar2=None, op0=ALU.mult), reads=["ex4", "rs"], writes=[f"gates{tbg}"])
                for k in range(4 if stage != "h1ns" else 0):
                    S.op("gp", I("indirect_dma_start", out=xs_scr,
                                 out_offset=bass.IndirectOffsetOnAxis(ap=idx_all[:, tbg * 4 + k:tbg * 4 + k + 1], axis=0),
                                 in_=rb, in_offset=None),
                         reads=[rk, f"idx{tbg}"], cwrites=["xs_scr"], dma=f"scat{tb % 2}")


        def T1_blocks(t, blks):
            for blk in blks:
                xb = xtok[:, blk % 2, :]
                xk = f"xtok{blk % 2}"
                S.op("sp", I("dma_start", out=xb, in_=xs[t, blk * 128:(blk + 1) * 128, :]), writes=[xk], dma=xk)
                for hb in range(2):
                    pb_, pk, _ = bank()
                    S.op("pe", transp4(pb_, [xb[:, (hb * 4 + c) * 128:(hb * 4 + c + 1) * 128] for c in range(4)]),
                         reads=[xk, "ident"], writes=[pk])
                    evac(R(xT[:, hb * 4:(hb + 1) * 4, blk * 128:(blk + 1) * 128]), r3(pb_[:, :], 4),
                         reads=[pk], cwrites=[f"xT{blk}"], eng=("act" if t > 0 else None))

        for t in range(NTILE):
            ld(kval[:], kval_d[t], "kval", sem="kval")
            ld(icnt[:], icnt_d[t], "icnt", sem="icnt")
            zero_fill()
            if t == 0:
                T1_blocks(0, range(6))
            xTall = [f"xT{b}" for b in range(6)]
            if STOP <= 1:
                break
            n_kv, s_kv, k_kv = pslot((t, "kv"))
            kvv = r3(s_kv[:, 0:2048], 8)
            for hh in range(2):
                pb_, pk, _ = bank()
                S.op("pe", mmgroup(pb_[:, 0:384], [(kvv[:, kc, 0:128], R(xT[:, kc, hh * 384:(hh + 1) * 384]))
                                                   for kc in range(8)]), reads=[k_kv] + xTall, writes=[pk])
                if SUB >= "b":
                    ee = "act" if hh == 0 else "dve"
                    evac(R(kT0[0:64, hh * 384:(hh + 1) * 384]), pb_[0:64, 0:384], reads=[pk], cwrites=["kT"], eng=ee)
                    evac(R(kT1[64:128, hh * 384:(hh + 1) * 384]), pb_[64:128, 0:384], reads=[pk], cwrites=["kT"], eng=ee)
            for half in range(2 if SUB >= "c" else 0):
                pb_, pk, _ = bank()
                calls = []
                for b3 in range(3):
                    blk = half * 3 + b3
                    for kc in range(8):
                        calls.append(("matmul", (pb_[:, b3 * 128:(b3 + 1) * 128], R(xT[:, kc, blk * 128:(blk + 1) * 128]),
                                                 kvv[:, kc, 128:256]), dict(start=(kc == 0), stop=(kc == 7))))
                S.op("pe", G(calls), reads=[k_kv] + xTall, writes=[pk])
                for b3 in range(3 if SUB >= "d" else 0):
                    blk = half * 3 + b3
                    evac(R(vaug[:, blk, :, 0:64]), r3(pb_[:, b3 * 128:(b3 + 1) * 128], 2), reads=[pk], cwrites=["vaug"],
                         eng=("act" if half == 0 else "dve"))
            piece_done(n_kv)
            for hd in range(2 if SUB >= "e" else 0):
                S.op("dve", I("tensor_copy", R(vaug[:, :, hd, 64]), kval[:, :]), reads=["kval"], cwrites=["vaug"])
            if STOP <= 2 or (STOP == 3 and SUB != ''):
                break
            n_q, s_q, k_q = pslot((t, "q"))
            qv = r3(s_q[:, 0:4096], 8)
            for i4 in range(4):
                pb_, pk, _ = bank()
                S.op("pe", mmgroup(pb_[:, :], [(qv[:, kc, i4 * 128:(i4 + 1) * 128], R(xT[:, kc, 128:640]))
                                               for kc in range(8)]), reads=[k_q] + xTall, writes=[pk])
                evac(R(qT[:, i4, :]), pb_[:, :], reads=[pk], cwrites=["qT"])
            piece_done(n_q)
            if STOP <= 3:
                break
            n_p, s_p, k_p = pslot((t, "p"))
            pv_ = r3(s_p[:, 0:4096], 8)
            for g in range(4):
                for hh in range(2):
                    pb_, pk, _ = bank()
                    S.op("pe", mmgroup(pb_[:, 0:272], [(pv_[:, kc, g * 128:(g + 1) * 128],
                                                        R(xT[:, kc, PW0 + hh * 272:PW0 + (hh + 1) * 272]))
                                                       for kc in range(8)]), reads=[k_p] + xTall, writes=[pk])
                    evac(pT[:, g, hh * 272:(hh + 1) * 272], pb_[:, 0:272], reads=[pk], cwrites=[f"pT{g}"])
            piece_done(n_p)
            if t > 0:
                router(t - 1)
            if not full and t == 0:
                S.op("sp", I("dma_start", out=dbg_qT, in_=qT.rearrange("p a b -> p (a b)")), reads=["qT"], dma="dbg")
                S.op("sp", I("dma_start", out=dbg_kT[:, 0:768], in_=kT0), reads=["kT"], dma="dbg")
                S.op("sp", I("dma_start", out=dbg_kT[:, 768:1536], in_=kT1), reads=["kT"], dma="dbg")
                S.op("sp", I("dma_start", out=dbg_v, in_=vaug.rearrange("p a b c -> p (a b c)")), reads=["vaug"], dma="dbg")
            if STOP <= 5:
                break
            pool_ops = []

            def PQ(eng, fn, **kw):
                pool_ops.append((eng, fn, kw))

            def pool_drip(n):
                for _ in range(n):
                    if pool_ops:
                        e_, f_, kw_ = pool_ops.pop(0)
                        S.op(e_, f_, **kw_)

            for g in range(4):
                cur = pT[:, g, :]
                curk = f"pT{g}"
                lo, hi = 8, PWN - 8
                for l in range(1, g + 2):
                    dst = ptmp[:, l % 2, :]
                    dk = f"ptmp{l % 2}"
                    if l == 1:
                        PQ("dve", I("tensor_tensor", out=dst[:, lo:hi], in0=cur[:, lo - 1:hi - 1], in1=cur[:, lo:hi],
                                      op=ALU.add), reads=[curk], writes=[dk])
                    else:
                        sh = 2 ** (l - 2)
                        PQ("dve", I("tensor_tensor", out=dst[:, lo:hi], in0=cur[:, lo - sh:hi - sh],
                                      in1=cur[:, lo + sh:hi + sh], op=ALU.add), reads=[curk], writes=[dk])
                    cur, curk = dst, dk
                w = 2 ** (g + 1)
                PQ("dve", I("scalar_tensor_tensor", out=R(yT[:, g, :]), in0=cur[:, 16:528], scalar=1.0 / w,
                              in1=pT[:, g, 16:528], op0=ALU.mult, op1=ALU.subtract),
                     reads=[curk, f"pT{g}"], writes=[f"yT{g}"])
                for ed in range(2):
                    u0 = 16 if ed == 0 else 520
                    y0 = 0 if ed == 0 else 504
                    ic = icnt[:, g * 16 + ed * 8:g * 16 + ed * 8 + 8]
                    PQ("dve", I("tensor_tensor", out=junk[:, 0:8], in0=cur[:, u0:u0 + 8], in1=ic, op=ALU.mult),
                         reads=[curk, "icnt"], writes=["junk"])
                    PQ("dve", I("tensor_tensor", out=R(yT[:, g, y0:y0 + 8]), in0=junk[:, 0:8], in1=pT[:, g, u0:u0 + 8],
                                  op=ALU.subtract), reads=["junk", f"pT{g}"], cwrites=[f"yT{g}"])
            if STOP <= 4:
                break
            pvbs = {}

            def att_S(qb, g, jj):
                if qb not in pvbs:
                    pvbs[qb] = [bank(), bank()]
                    reserved.update([pvbs[qb][0][2], pvbs[qb][1][2]])
                km = kT0 if g == 0 else kT1
                kb = qb + jj
                pb_, pk, _ = bank()
                S.op("pe", I("matmul", pb_[:, :], R(km[:, kb * 128:(kb + 1) * 128]), R(qT[:, :, qb * 128:(qb + 1) * 128]),
                             start=True, stop=True), reads=["kT", "qT"], writes=[pk])
                pp = (qb * 6 + g * 3 + jj) % 2
                S.op("act", I("activation", out=expS2[:, pp, :], in_=pb_[:, :], func=AF.Exp, scale=0.125),
                     reads=[pk], writes=[f"expS{pp}"])
                S.op("dve", I("tensor_tensor", out=r3(R(PT2[:, pp, :]), 4), in0=r3(expS2[:, pp, :], 4),
                              in1=ebias[:, jj, 4 * g:4 * g + 4, :], op=ALU.mult), reads=[f"expS{pp}", "ebias"], writes=[f"PT{pp}"])

            def att_PV(qb, g, jj):
                pp = (qb * 6 + g * 3 + jj) % 2
                PT = PT2[:, pp, :]
                pvt, pvk, _ = pvbs[qb][g]
                calls = [("matmul", (pvt[:, i * 66:(i + 1) * 66], R(PT[:, i * 128:(i + 1) * 128]), R(vaug[:, qb + jj, g, :])),
                          dict(start=(jj == 0 and i == 0), stop=(jj == 2 and i == 3))) for i in range(4)]
                if jj == 0:
                    S.op("pe", G(calls), reads=[f"PT{pp}", "vaug"], writes=[pvk])
                else:
                    S.op("pe", G(calls), reads=[f"PT{pp}", "vaug"], cwrites=[pvk])

            def att_N(qb):
                pvb = pvbs[qb]
                for hb in range(2):
                    pvt, pvk, _ = pvb[hb]
                    S.op("dve", I("tensor_tensor", out=den[:, hb * 4:(hb + 1) * 4], in0=r3(pvt[:, 0:264], 4)[:, :, 64],
                                  in1=esink[:, hb * 4:(hb + 1) * 4], op=ALU.add), reads=[pvk, "esink"], cwrites=["den"])
                S.op("dve", I("reciprocal", out=rec, in_=den), reads=["den"], writes=["rec"])
                for hd in range(8):
                    pvt, pvk, _ = pvb[hd // 4]
                    c0 = (hd % 4) * 66
                    if hd // 4 == 0:
                        S.op("act", I("activation", out=attn_tok[:, hd * 64:(hd + 1) * 64], in_=pvt[:, c0:c0 + 64],
                                      func=AF.Identity, scale=rec[:, hd:hd + 1]), reads=[pvk, "rec"], cwrites=["attn_tok"])
                    else:
                        S.op("dve", I("tensor_scalar", out=attn_tok[:, hd * 64:(hd + 1) * 64], in0=pvt[:, c0:c0 + 64],
                                      scalar1=rec[:, hd:hd + 1], scalar2=None, op0=ALU.mult),
                             reads=[pvk, "rec"], cwrites=["attn_tok"])
                reserved.discard(pvb[0][2])
                reserved.discard(pvb[1][2])

            def att_T(qb):
                pb_, pk, _ = bank()
                S.op("pe", transp4(pb_, [attn_tok[:, c * 128:(c + 1) * 128] for c in range(4)]),
                     reads=["attn_tok", "ident"], writes=[pk])
                evac(R(attnT[:, :, qb * 128:(qb + 1) * 128]), r3(pb_[:, :], 4), reads=[pk], cwrites=["attnT"])

            steps = [(qb, g, jj) for qb in range(4) for g in range(2) for jj in range(3)]
            att_S(*steps[0])
            for si, (qb, g, jj) in enumerate(steps):
                if si + 1 < len(steps):
                    att_S(*steps[si + 1])
                att_PV(qb, g, jj)
                pool_drip(2)
                if g == 0 and jj == 1 and qb > 0:
                    att_T(qb - 1)
                if g == 1 and jj == 2:
                    att_N(qb)
            att_T(3)
            pool_drip(1000)
            if not full and t == 0:
                S.op("sp", I("dma_start", out=dbg_yT, in_=yT.rearrange("p a b -> p (a b)")), reads=[f"yT{g_}" for g_ in range(4)], dma="dbg")
            if STOP <= 6:
                break
            n_pg, s_pg, k_pg = pslot((t, "pg"))
            pgv = r3(s_pg[:, 0:512], 4)
            for g in range(4):
                pb_, pk, _ = bank()
                S.op("pe", I("matmul", pb_[:, :], pgv[:, g, :], R(yT[:, g, :]), start=True, stop=True),
                     reads=[k_pg, f"yT{g}"], writes=[pk])
                S.op("act", I("activation", out=R(zsT[:, g, :]), in_=pb_[:, :], func=AF.Identity, scale=pscale[:, g:g + 1]),
                     reads=[pk, "pscale"], cwrites=["zsT"])
            piece_done(n_pg)
            if STOP <= 7:
                break
            for j in range(8):
                n_g, s_g, k_g = pslot((t, "g", j // 2))
                n_b, s_b, k_b = pslot((t, "br", j // 4))
                cb = (j % 4) * 128
                cg = (j % 2) * 128
                abv = r3(s_b[:, 0:2048], 4)[:, :, cb:cb + 128]
                pbv = r3(s_b[:, 2048:4096], 4)[:, :, cb:cb + 128]
                gav = r3(s_g[:, 0:2048], 8)[:, :, cg:cg + 128]
                gpv = r3(s_g[:, 2048:4096], 8)[:, :, cg:cg + 128]
                bA, kA, _ = bank(); bB, kB, _ = bank(); bC, kC, _ = bank(); bD, kD, _ = bank()
                S.op("pe", mmgroup(bC[:, :], [(gav[:, kc, :], R(xT[:, kc, 128:640])) for kc in range(8)]),
                     reads=[k_g] + xTall, writes=[kC])
                S.op("pe", mmgroup(bD[:, :], [(gpv[:, kc, :], R(xT[:, kc, 128:640])) for kc in range(8)]),
                     reads=[k_g] + xTall, writes=[kD])
                S.op("pe", mmgroup(bA[:, :], [(abv[:, c, :], R(attnT[:, c, :])) for c in range(4)]),
                     reads=[k_b, "attnT"], writes=[kA])
                S.op("pe", mmgroup(bB[:, :], [(pbv[:, c, :], R(zsT[:, c, :])) for c in range(4)]),
                     reads=[k_b, "zsT"], writes=[kB])
                if j % 2 == 1:
                    piece_done(n_g)
                if j % 4 == 3:
                    piece_done(n_b)
                S.op("act", I("activation", out=sa, in_=bC[:, :], func=AF.Sigmoid), reads=[kC], writes=["sa"])
                S.op("act", I("activation", out=spb, in_=bD[:, :], func=AF.Sigmoid), reads=[kD], writes=["spb"])
                S.op("dve", I("tensor_tensor", out=sa, in0=sa, in1=bA[:, :], op=ALU.mult), reads=["sa", kA], writes=["sa"])
                S.op("dve", I("tensor_tensor", out=spb, in0=spb, in1=bB[:, :], op=ALU.mult), reads=["spb", kB], writes=["spb"])
                S.op("dve", I("tensor_tensor", out=R(mT[:, j, :]), in0=sa, in1=spb, op=ALU.add),
                     reads=["sa", "spb"], cwrites=["mT"])
            if not full and t == 0:
                S.op("sp", I("dma_start", out=dbg_attnT, in_=attnT.rearrange("p a b -> p (a b)")), reads=["attnT"], dma="dbg")
                S.op("sp", I("dma_start", out=dbg_zsT, in_=zsT.rearrange("p a b -> p (a b)")), reads=["zsT"], dma="dbg")
                S.op("sp", I("dma_start", out=dbg_mT, in_=mT.rearrange("p a b -> p (a b)")), reads=["mT"], dma="dbg")
            if STOP <= 8:
                break
            n_w0, s_w0, k_w0 = pslot((t, "wo", 0))
            n_w1, s_w1, k_w1 = pslot((t, "wo", 1))
            wov = [r3(s_w0[:, 0:4096], 8), r3(s_w1[:, 0:4096], 8)]
            wok = [k_w0, k_w1]
            def stageA(tb):
                rb = r1[:, tb % 3, :]
                rk = f"r1_{tb % 3}"
                S.op("gp", I("dma_start", out=rb, in_=xs[t, 128 + tb * 128:128 + (tb + 1) * 128, :]),
                     writes=[rk], dma=rk + "ld")
                for nh in range(2):
                    pb_, pk, _ = bank()
                    S.op("pe", mmgroup(pb_[:, :], [(R(mT[:, j, tb * 128:(tb + 1) * 128]), wov[nh][:, j, :]) for j in range(8)]),
                         reads=[wok[nh], "mT"], writes=[pk])
                    S.op("dve", I("scalar_tensor_tensor", out=rb[:, nh * 512:(nh + 1) * 512], in0=rb[:, nh * 512:(nh + 1) * 512],
                                  scalar=ALPHA, in1=pb_[:, :], op0=ALU.mult, op1=ALU.add), reads=[pk, rk], cwrites=[rk])
                if tb == 3:
                    piece_done(n_w0)
                    piece_done(n_w1)

            stageA(0)
            for tb in range(4):
                tbg = t * 4 + tb
                rb = r1[:, tb % 3, :]
                rk = f"r1_{tb % 3}"
                if tb + 1 < 4:
                    stageA(tb + 1)
                layer_norm_tok(rb, ln1g, ln1b, rb, ntmp1, rk, rk, "ln1g", "ln1b")
                S.op("gp", I("dma_start", out=h1_scr[tbg * 128:(tbg + 1) * 128, :], in_=rb),
                     reads=[rk], cwrites=["h1_scr"], dma=rk + "st")
                if t + 1 < NTILE and STOP == 99:
                    T1_blocks(t + 1, {0: [0, 1], 1: [2, 3], 2: [4], 3: [5]}[tb])
            if t == NTILE - 1:
                router(t)

        if not full and STOP == 99:
            S.op("sp", I("dma_start", out=dbg_idx, in_=idx_all[:]), reads=[f"idx{i}" for i in range(16)], dma="dbg")
            S.op("sp", I("dma_start", out=dbg_gates, in_=gates_all[:]), reads=[f"gates{i}" for i in range(16)], dma="dbg")

        if full:
            S.barrier()
            offs["r"], offs["f"] = 0, 0
            xs_tok = tF([128, 3, D])
            xsT = tR([128, 8, CAP])
            actT = tR([128, 8, CAP])
            gbuf = tF([128, 2, CAP])
            sgb = tF([128, 2, CAP])
            ubuf = tF([128, 2, CAP])
            y_tok = tF([128, 3, D])
            b2bc = tF([128, 2, D])
            E0 = tR([128, 128])
            b2pad = tR([128, 2, D])
            assert offs["r"] <= XOFF

            def load_xs(e):
                S.op("sp", I("dma_start", out=xs_tok,
                             in_=xs_scr[e * CAP:(e + 1) * CAP, :].rearrange("(b p) d -> p b d", p=128)),
                     reads=["xs_scr"], writes=["xs_tok"], dma="xs_tok")

            def transposes(e):
                for kc in range(8):
                    pb_, pk, _ = bank()
                    S.op("pe", G([("transpose", (), dict(out=pb_[:, b * 128:(b + 1) * 128],
                                                         in_=xs_tok[:, b, kc * 128:(kc + 1) * 128], identity=ident[:]))
                                  for b in range(3)]), reads=["xs_tok", "ident"], writes=[pk])
                    evac(R(xsT[:, kc, :]), pb_[:, 0:CAP], reads=[pk], cwrites=["xsT"], eng="act")

            def mlp1(e):
                S.op("gp", I("dma_start", out=R(b2pad[0:1, e % 2, :]), in_=b2_d[e:e + 1, :]),
                     writes=[f"b2pad{e % 2}"], dma=f"b2pad{e % 2}")
                for cg_ in range(2):
                    n_a, s_a, k_a = pslot((e, "w1", cg_, 0))
                    n_b, s_b, k_b = pslot((e, "w1", cg_, 1))
                    wh = [s_a[:, 0:4096].rearrange("p (kc t c) -> p kc t c", kc=4, t=2),
                          s_b[:, 0:4096].rearrange("p (kc t c) -> p kc t c", kc=4, t=2)]
                    for ii in range(4):
                        i8 = cg_ * 4 + ii
                        bG, kG, _ = bank(); bU, kU, _ = bank()
                        S.op("pe", mmgroup(bG[:, 0:CAP], [(wh[kc // 4][:, kc % 4, 0, ii * 128:(ii + 1) * 128], R(xsT[:, kc, :]))
                                                          for kc in range(8)]), reads=[k_a, k_b, "xsT"], writes=[kG])
                        S.op("pe", mmgroup(bU[:, 0:CAP], [(wh[kc // 4][:, kc % 4, 1, ii * 128:(ii + 1) * 128], R(xsT[:, kc, :]))
                                                          for kc in range(8)]), reads=[k_a, k_b, "xsT"], writes=[kU])
                        p2 = i8 % 2
                        gb = gbuf[:, p2, :]; sg = sgb[:, p2, :]; ub = ubuf[:, p2, :]
                        cg = e * 16 + i8
                        cu = e * 16 + 8 + i8
                        S.op("dve", I("tensor_scalar", out=gb, in0=bG[:, 0:CAP], scalar1=b1T[:, cg:cg + 1], scalar2=7.0,
                                      op0=ALU.add, op1=ALU.min), reads=[kG, "b1T"], writes=[f"gb{p2}"])
                        S.op("act", I("activation", out=sg, in_=gb, func=AF.Sigmoid, scale=1.702),
                             reads=[f"gb{p2}"], writes=[f"sg{p2}"])
                        S.op("dve", I("tensor_scalar", out=ub, in0=bU[:, 0:CAP], scalar1=b1T[:, cu:cu + 1], scalar2=7.0,
                                      op0=ALU.add, op1=ALU.min), reads=[kU, "b1T"], writes=[f"ub{p2}"])
                        S.op("dve", I("tensor_scalar", out=ub, in0=ub, scalar1=-7.0, scalar2=1.0, op0=ALU.max, op1=ALU.add),
                             reads=[f"ub{p2}"], writes=[f"ub{p2}"])
                        S.op("dve", I("tensor_tensor", out=gb, in0=gb, in1=sg, op=ALU.mult),
                             reads=[f"gb{p2}", f"sg{p2}"], writes=[f"gb{p2}"])
                        S.op("dve", I("tensor_tensor", out=R(actT[:, i8, :]), in0=ub, in1=gb, op=ALU.mult),
                             reads=[f"gb{p2}", f"ub{p2}"], cwrites=["actT"])
                    piece_done(n_a)
                    piece_done(n_b)

            def mlp2(e):
                n_a, s_a, k_a = pslot((e, "w2", 0))
                n_b, s_b, k_b = pslot((e, "w2", 1))
                w2v = [r3(s_a[:, 0:4096], 4), r3(s_b[:, 0:4096], 4)]
                bb = b2bc[:, e % 2, :]
                bk = f"b2bc{e % 2}"
                for nh in range(2):
                    pbb, pkb, _ = bank()
                    S.op("pe", I("matmul", pbb[:, :], R(E0[:, :]), R(b2pad[:, e % 2, nh * 512:(nh + 1) * 512]), start=True, stop=True),
                         reads=["E0", f"b2pad{e % 2}"], writes=[pkb])
                    kwb = dict(writes=[bk]) if nh == 0 else dict(cwrites=[bk])
                    S.op("act", I("copy", out=bb[:, nh * 512:(nh + 1) * 512], in_=pbb[:, :]), reads=[pkb], **kwb)
                for b in range(3):
                    yb = y_tok[:, b, :]
                    yk = f"ytok{b}"
                    for nh in range(2):
                        pb_, pk, _ = bank()
                        S.op("pe", mmgroup(pb_[:, :], [(R(actT[:, fc, b * 128:(b + 1) * 128]),
                                                        w2v[fc // 4][:, fc % 4, nh * 512:(nh + 1) * 512]) for fc in range(8)]),
                             reads=[k_a, k_b, "actT"], writes=[pk])
                        kw = dict(writes=[yk]) if nh == 0 else dict(cwrites=[yk])
                        S.op("dve", I("tensor_tensor", out=yb[:, nh * 512:(nh + 1) * 512], in0=pb_[:, :],
                                      in1=bb[:, nh * 512:(nh + 1) * 512], op=ALU.add), reads=[pk, bk], **kw)
                    S.op("sp", I("dma_start", out=ys_scr[e * CAP + b * 128:e * CAP + (b + 1) * 128, :], in_=yb),
                         reads=[yk], cwrites=["ys_scr"], dma=yk)
                piece_done(n_a)
                piece_done(n_b)

            for n in range(n0_moe, len(pieces)):
                if slot_of[n] >= NRING and (n - n0_moe) < NR2:
                    emit_piece_load(n)
            S.op("dve", I("memset", y_tok[:, 0, :], 0.0), writes=["ytok0"])
            S.op("dve", I("memset", y_tok[:, 1, :], 0.0), writes=["ytok1"])
            for i_ in range(2):
                S.op("dve", I("tensor_copy", R(b2pad[:, i_, :]), y_tok[:, i_, :]), reads=[f"ytok{i_}"], writes=[f"b2pad{i_}"])
            S.op("dve", I("tensor_copy", y_tok[0:1, 1, 0:128], ones[0:1, :]), reads=["ones"], writes=["ytok1"])
            S.op("dve", I("tensor_copy", R(E0[:, :]), y_tok[:, 1, 0:128]), reads=["ytok1"], writes=["E0"])
            S.op("sp", I("dma_start", out=ys_scr[NE * CAP:NE * CAP + 128, :], in_=y_tok[:, 0, :]),
                 reads=["ytok0"], cwrites=["ys_scr"], dma="ytok0")
            load_xs(0)
            transposes(0)
            for e in range(NE):
                mlp1(e)
                if e + 1 < NE:
                    load_xs(e + 1)
                    transposes(e + 1)
                mlp2(e)

            S.barrier()
            offs["r"], offs["f"] = 0, 0
            yg = tR([128, 2, 4, D])
            dg = tR([128, 2, 4, 128])
            ln2g = tF([128, D])
            ln2b = tF([128, D])
            hb2 = tF([128, 2, D])
            ob2 = tF([128, 2, D])
            ld(ln2g, ln2g_d, "ln2g")
            ld(ln2b, ln2b_d, "ln2b")

            def gathers(tbg):
                p2 = tbg % 2
                for k in range(4):
                    kw = dict(writes=[f"yg{p2}"]) if k == 0 else dict(cwrites=[f"yg{p2}"])
                    S.op("gp", I("indirect_dma_start", out=R(yg[:, p2, k, :]), out_offset=None, in_=ys_scr,
                                 in_offset=bass.IndirectOffsetOnAxis(ap=idx_all[:, tbg * 4 + k:tbg * 4 + k + 1], axis=0)),
                         reads=["ys_scr", f"idx{tbg}"], dma=f"yg{p2}", **kw)
                S.op("sp", I("dma_start", out=hb2[:, p2, :], in_=h1_scr[tbg * 128:(tbg + 1) * 128, :]),
                     reads=["h1_scr"], writes=[f"hb{p2}"], dma=f"hb{p2}")

            def combine_mm(tbg):
                p2 = tbg % 2
                for k in range(4):
                    kw = dict(writes=[f"dg{p2}"]) if k == 0 else dict(cwrites=[f"dg{p2}"])
                    S.op("dve", I("tensor_scalar", out=R(dg[:, p2, k, :]), in0=ident[:],
                                  scalar1=gates_all[:, tbg * 4 + k:tbg * 4 + k + 1], scalar2=None, op0=ALU.mult),
                         reads=["ident", f"gates{tbg}"], **kw)
                res = []
                for nh in range(2):
                    pb_, pk, bi = bank()
                    reserved.add(bi)
                    S.op("pe", mmgroup(pb_[:, :], [(R(dg[:, p2, k, :]), R(yg[:, p2, k, nh * 512:(nh + 1) * 512])) for k in range(4)]),
                         reads=[f"dg{p2}", f"yg{p2}"], writes=[pk])
                    res.append((pb_, pk, bi))
                return res

            gathers(0)
            gathers(1)
            pend = combine_mm(0)
            for tbg in range(16):
                p2 = tbg % 2
                cur = pend
                if tbg + 1 < 16:
                    pend = combine_mm(tbg + 1)
                hb = hb2[:, p2, :]
                hk = f"hb{p2}"
                for nh in range(2):
                    pb_, pk, bi = cur[nh]
                    S.op("dve", I("scalar_tensor_tensor", out=hb[:, nh * 512:(nh + 1) * 512], in0=hb[:, nh * 512:(nh + 1) * 512],
                                  scalar=ALPHA, in1=pb_[:, :], op0=ALU.mult, op1=ALU.add), reads=[pk, hk], cwrites=[hk])
                    reserved.discard(bi)
                ob = ob2[:, p2, :]
                ok_ = f"ob{p2}"
                layer_norm_tok(hb, ln2g, ln2b, ob, ob, hk, ok_, "ln2g", "ln2b", nkey=ok_)
                if tbg + 2 < 16:
                    gathers(tbg + 2)
                S.op("sp", I("dma_start", out=out_d[tbg * 128:(tbg + 1) * 128, :], in_=ob),
                     reads=[ok_], cwrites=["out"], dma=f"outst{p2}")

        S.final_waits("sp")

        @block.sync
        def _(h):
            S.replay("sp", h)

        @block.tensor
        def _(h):
            S.replay("pe", h)

        @block.scalar
        def _(h):
            S.replay("act", h)

        @block.vector
        def _(h):
            S.replay("dve", h)

        @block.gpsimd
        def _(h):
            S.replay("gp", h)
    return nc


def _consts():
    slopes = np.asarray([2.0 ** (-8.0 * (h + 1) / 8) for h in range(8)], np.float64)
    kk = np.arange(128)[:, None]
    qq = np.arange(128)[None, :]
    eb = np.zeros((128, 3, 8, 128), np.float64)
    for jj in range(3):
        rel = kk + (jj - 1) * 128 - qq
        valid = np.abs(rel) <= 128
        for h in range(8):
            eb[:, jj, h, :] = np.where(valid, np.exp(-slopes[h] * np.abs(rel)), 0.0)
    ident = np.eye(128, dtype=np.float32)
    tri = (np.arange(128)[:, None] < np.arange(128)[None, :]).astype(np.float32)
    ones = np.ones((128, 128), np.float32)
    ecol = np.broadcast_to((np.arange(NE) * CAP).astype(np.float32)[None, :], (128, NE)).copy()
    return dict(ebias=eb.reshape(128, 3072).astype(np.float32), ident=ident, tri=tri, ones=ones, ecol=ecol)


def _bc(v):
    return np.ascontiguousarray(np.broadcast_to(np.asarray(v, np.float32).reshape(1, -1), (128, v.size)))


def make_in_maps(inputs, stage="full"):
    x = np.asarray(inputs["x"], np.float32)
    cst = _consts()
    shared = dict(cst)
    shared["sinks"] = _bc(inputs["attn_sinks"][0])
    shared["ln1g"] = _bc(inputs["ln1_g"][0]); shared["ln1b"] = _bc(inputs["ln1_b"][0])
    shared["ln2g"] = _bc(inputs["ln2_g"][0]); shared["ln2b"] = _bc(inputs["ln2_b"][0])
    shared["pscale"] = np.ascontiguousarray(np.asarray(inputs["pool_scale"][0], np.float32).reshape(4, 128).T)
    shared["brout"] = _bc(inputs["b_router"][0])
    shared["b1T"] = np.ascontiguousarray(
        np.asarray(inputs["b_mlp1"][0], np.float32).reshape(NE, 16, 128).transpose(2, 0, 1).reshape(128, NE * 16))
    shared["b2"] = np.ascontiguousarray(np.asarray(inputs["b_mlp2"][0], np.float32))
    shared["wr"] = np.ascontiguousarray(np.asarray(inputs["w_router"][0], np.float32))
    shared["w_in"] = np.ascontiguousarray(np.asarray(inputs["w_in"][0], np.float32))
    shared["wab"] = np.ascontiguousarray(np.asarray(inputs["w_attn_branch"][0], np.float32))
    shared["wpg"] = np.ascontiguousarray(np.asarray(inputs["w_pool_group"][0], np.float32).reshape(512, 128))
    shared["wpb"] = np.ascontiguousarray(np.asarray(inputs["w_pool_branch"][0], np.float32))
    shared["wout"] = np.ascontiguousarray(np.asarray(inputs["w_out"][0], np.float32))
    shared["w1"] = np.ascontiguousarray(np.asarray(inputs["w_mlp1"][0], np.float32))
    shared["w2"] = np.ascontiguousarray(np.asarray(inputs["w_mlp2"][0], np.float32))
    maps = []
    for c in range(NCORES):
        b = c // 4
        s0 = (c % 4) * TOK
        xs = np.zeros((NTILE, SLAB, D), np.float32)
        kval = np.zeros((NTILE, 128, 6), np.float32)
        icnt = np.zeros((NTILE, 128, 64), np.float32)
        for t in range(NTILE):
            st = s0 + t * TT - 128
            lo, hi = max(st, 0), min(st + SLAB, SEQ)
            xs[t, lo - st:hi - st] = x[b, lo:hi]
            pos = st + np.arange(SLAB)
            v = ((pos >= 0) & (pos < SEQ)).astype(np.float32)
            kval[t] = v.reshape(6, 128).T
            for g, w in enumerate((2, 4, 8, 16)):
                for ed in range(2):
                    tt = s0 + t * TT + (np.arange(8) if ed == 0 else 504 + np.arange(8))
                    lo_ = np.maximum(tt - w // 2, 0)
                    hi_ = np.minimum(tt + w // 2 - 1, SEQ - 1)
                    icnt[t, :, g * 16 + ed * 8:g * 16 + ed * 8 + 8] = (1.0 / (hi_ - lo_ + 1)).astype(np.float32)[None, :]
        m = dict(shared)
        m["xs"] = xs; m["kval"] = kval; m["icnt"] = icnt
        if stage != "full":
            for k in ("ln2g", "ln2b", "b1T", "b2", "w1", "w2"):
                m.pop(k)
        maps.append(m)
    return maps


def kernel(**inputs):
    nc = build_program("full")
    maps = make_in_maps(inputs)
    res = run_bass_kernel_spmd(nc, maps, core_ids=list(range(NCORES)))
    outs = [np.asarray(r["out"], np.float32) for r in res.results]
    full = np.concatenate(outs, axis=0).reshape(2, SEQ, D)
    return full
```

```python
import numpy as np
import concourse.bass as bass
import concourse.mybir as mybir
from concourse.bass_utils import run_bass_kernel_spmd

F32 = mybir.dt.float32
F32R = mybir.dt.float32r
I32 = mybir.dt.int32
AF = mybir.ActivationFunctionType
ALU = mybir.AluOpType
AX = mybir.AxisListType

NCORES = 8
D = 1024
SEQ = 8192
TOK = 2048
TT = 512
NTILE = 4
SLAB = 768
NE = 32
CAP = 384
PW0 = 112
PWN = 544
ALPHA = 2.0 ** 0.25
LN_EPS = 1e-5
RSLOT = 4096
NRING = 5


class Sched:
    def __init__(self, sems):
        self.pool_sems = list(sems)
        self.eng = {}
        for n in ("pe", "act", "dve", "gp", "sp"):
            self.eng[n] = dict(sem=self.pool_sems.pop(), count=0, prog=[], seen={}, pend={})
        self.dsem = {}
        self.buf = {}
        self.fence = {}

    def _b(self, k):
        if k not in self.buf:
            self.buf[k] = dict(w={}, r={})
        return self.buf[k]

    @staticmethod
    def _mx(d, tok):
        s, v = tok
        key = id(s)
        if key not in d or d[key][1] < v:
            d[key] = (s, v)

    def op(self, eng, fn, reads=(), writes=(), cwrites=(), dma=None):
        E = self.eng[eng]
        fr, fw = set(), set()
        for k in list(reads) + list(writes) + list(cwrites):
            if k in self.fence:
                fk, mode = self.fence[k]
                (fr if mode == "X" else fw).add(fk)
        if fr or fw:
            reads = list(reads) + sorted(fr)
            cwrites = list(cwrites) + sorted(fw)
        deps = {}
        raw = {}
        for k in reads:
            for t in self._b(k)["w"].values():
                self._mx(deps, t)
                self._mx(raw, t)
        for k in writes:
            b = self._b(k)
            for t in b["w"].values():
                self._mx(deps, t)
            for t in b["r"].values():
                self._mx(deps, t)
        for k in cwrites:
            for t in self._b(k)["r"].values():
                self._mx(deps, t)
        for t in E["pend"].values():
            self._mx(deps, t)
        E["pend"] = {}
        waits = []
        for key, (s, v) in deps.items():
            if s is E["sem"]:
                if eng == "pe" or key not in raw:
                    continue
                v = raw[key][1]
            if E["seen"].get(key, 0) >= v:
                continue
            E["seen"][key] = v
            waits.append((s, v))
        if dma is None:
            E["count"] += 1
            tok = (E["sem"], E["count"])
            inc = (E["sem"], 1)
        else:
            if dma not in self.dsem:
                self.dsem[dma] = [self.pool_sems.pop(), 0]
            ds = self.dsem[dma]
            ds[1] += 16
            tok = (ds[0], ds[1])
            inc = (ds[0], 16)
        E["prog"].append((waits, fn, inc))
        for k in reads:
            self._mx(self._b(k)["r"], tok)
        for k in writes:
            b = self._b(k)
            b["w"] = {}
            b["r"] = {}
            self._mx(b["w"], tok)
        for k in cwrites:
            self._mx(self._b(k)["w"], tok)
        return tok

    def barrier(self):
        toks = [(e["sem"], e["count"]) for e in self.eng.values() if e["count"] > 0]
        toks += [(d[0], d[1]) for d in self.dsem.values() if d[1] > 0]
        for e in self.eng.values():
            for t in toks:
                if t[0] is not e["sem"]:
                    self._mx(e["pend"], t)

    def final_waits(self, eng):
        E = self.eng[eng]
        toks = [(e["sem"], e["count"]) for n, e in self.eng.items() if e["count"] > 0 and n != eng]
        toks += [(d[0], d[1]) for d in self.dsem.values() if d[1] > 0]
        E["final"] = toks

    def replay(self, eng, h):
        E = self.eng[eng]
        for waits, fn, (s, n) in E["prog"]:
            for (ws, wv) in waits:
                h.wait_ge(ws, wv)
            inst = fn(h)
            inst.then_inc(s, n)
        for (ws, wv) in E.get("final", []):
            h.wait_ge(ws, wv)


def I(m, *a, **k):
    return lambda h: getattr(h, m)(*a, **k)


def G(calls):
    def f(h):
        r = None
        for (m, a, k) in calls:
            r = getattr(h, m)(*a, **k)
        return r
    return f


def build_program(stage="full"):
    nc = bass.Bass("TRN2", target_bir_lowering=False)
    full = stage == "full"
    STOP = int(stage[1:2]) if stage.startswith("s") else 99
    SUB = stage[2:] if stage.startswith("s") else "z"

    def din(name, shape, dt=F32):
        return nc.dram_tensor(name, list(shape), dt, kind="ExternalInput").ap()

    xs = din("xs", [NTILE, SLAB, D])
    kval_d = din("kval", [NTILE, 128, 6])
    icnt_d = din("icnt", [NTILE, 128, 64])
    ebias_d = din("ebias", [128, 3072])
    ident_d = din("ident", [128, 128])
    tri_d = din("tri", [128, 128])
    ones_d = din("ones", [128, 128])
    ecol_d = din("ecol", [128, NE])
    sinks_d = din("sinks", [128, 8])
    ln1g_d = din("ln1g", [128, D])
    ln1b_d = din("ln1b", [128, D])
    pscale_d = din("pscale", [128, 4])
    brout_d = din("brout", [128, NE])
    wr_d = din("wr", [D, NE])
    w_in = din("w_in", [D, 3328])
    wab = din("wab", [512, D])
    wpg = din("wpg", [512, 128])
    wpb = din("wpb", [512, D])
    wout = din("wout", [D, D])
    if full:
        ln2g_d = din("ln2g", [128, D])
        ln2b_d = din("ln2b", [128, D])
        b1T_d = din("b1T", [128, NE * 16])
        b2_d = din("b2", [NE, D])
        w1 = din("w1", [NE, D, 2 * D])
        w2 = din("w2", [NE, D, D])
        out_d = nc.dram_tensor("out", [TOK, D], F32, kind="ExternalOutput").ap()
    xs_scr = nc.dram_tensor("xs_scr", [NE * CAP + 128, D], F32, kind="Internal").ap()
    ys_scr = nc.dram_tensor("ys_scr", [NE * CAP + 128, D], F32, kind="Internal").ap()
    h1_scr = nc.dram_tensor("h1_scr", [TOK, D], F32, kind="Internal" if full else "ExternalOutput").ap()
    if not full:
        dbg_attnT = nc.dram_tensor("dbg_attnT", [128, 2048], F32, kind="ExternalOutput").ap()
        dbg_zsT = nc.dram_tensor("dbg_zsT", [128, 2048], F32, kind="ExternalOutput").ap()
        dbg_mT = nc.dram_tensor("dbg_mT", [128, 4096], F32, kind="ExternalOutput").ap()
        dbg_qT = nc.dram_tensor("dbg_qT", [128, 2048], F32, kind="ExternalOutput").ap()
        dbg_kT = nc.dram_tensor("dbg_kT", [128, 1536], F32, kind="ExternalOutput").ap()
        dbg_v = nc.dram_tensor("dbg_v", [128, 792], F32, kind="ExternalOutput").ap()
        dbg_yT = nc.dram_tensor("dbg_yT", [128, 2048], F32, kind="ExternalOutput").ap()
        dbg_idx = nc.dram_tensor("dbg_idx", [128, 64], I32, kind="ExternalOutput").ap()
        dbg_gates = nc.dram_tensor("dbg_gates", [128, 64], F32, kind="ExternalOutput").ap()

    WNR = 17688
    WNF = 12992
    from contextlib import ExitStack
    with ExitStack() as es:
        def sb(name, shape, dt=F32):
            return es.enter_context(nc.sbuf_tensor(name, list(shape), dt))

        workr = sb("workr", [128, WNR])
        workf = sb("workf", [128, WNF])
        ring_t = sb("ring", [128, NRING * RSLOT], F32R)
        ident = sb("ident_s", [128, 128])
        tri = sb("tri_s", [128, 128])
        ones = sb("ones_s", [128, 128])
        ecol = sb("ecol_s", [128, NE])
        esink = sb("esink", [128, 8])
        pscale = sb("pscale_s", [128, 4])
        brout = sb("brout_s", [128, NE])
        b1T = sb("b1T_s", [128, NE * 16])
        wr = sb("wr_s", [128, 8 * NE])
        runcnt = sb("runcnt", [128, NE])
        gates_all = sb("gates_all", [128, 16 * 4])
        idx_all = sb("idx_all", [128, 16 * 4], I32)
        kval = sb("kval_s", [128, 6])
        icnt = sb("icnt_s", [128, 64])
        small = sb("small", [128, 256])
        psb = [es.enter_context(nc.psum_tensor(f"ps{i}", [128, 512], F32)) for i in range(8)]
        sems = [es.enter_context(nc.semaphore(f"s{i}")) for i in range(80)]
        S = Sched(sems)
        for k_ in ["qT", "kT", "vaug", "PT0", "PT1"] + [f"yT{g_}" for g_ in range(4)]:
            S.fence[k_] = ("FXr", "X")
        S.fence["mT"] = ("FXr", "Y")
        for k_ in ["expS0", "expS1", "attn_tok", "ptmp0", "ptmp1", "h1T"] + [f"pT{g_}" for g_ in range(4)]:
            S.fence[k_] = ("FXf", "X")
        for k_ in ["sa", "spb", "r1_0", "r1_1", "r1_2", "ntmp"]:
            S.fence[k_] = ("FXf", "Y")
        block = es.enter_context(nc.Block())

        def carve(work, off, shape):
            n = int(np.prod(shape[1:]))
            ap = work[:, off:off + n]
            if len(shape) == 3:
                ap = ap.rearrange("p (a b) -> p a b", a=shape[1])
            elif len(shape) == 4:
                ap = ap.rearrange("p (a b c) -> p a b c", a=shape[1], b=shape[2])
            return ap

        def R(ap):
            return ap.bitcast(F32R)

        def r3(ap, a):
            return ap.rearrange("p (a b) -> p a b", a=a)

        psn = [0]
        reserved = set()

        def bank():
            while True:
                i = psn[0] % 8
                psn[0] += 1
                if i not in reserved:
                    return psb[i], f"ps{i}", i

        NR2 = 7
        XOFF = 8320

        def ring(slot):
            if slot < NRING:
                return ring_t[:, slot * RSLOT:(slot + 1) * RSLOT]
            o_ = XOFF + (slot - NRING) * RSLOT
            return workr[:, o_:o_ + RSLOT].bitcast(F32R)

        pieces = []

        slot_of = {}
        next_on_slot = {}

        def emit_piece_load(n):
            slot = slot_of[n]
            sl = ring(slot)
            first = True
            for (dfn, src) in pieces[n]:
                dst = dfn(sl)
                kw = dict(writes=[f"ring{slot}"]) if first else dict(cwrites=[f"ring{slot}"])
                S.op("gp", I("dma_start", out=dst, in_=src), dma=f"ring{slot}", **kw)
                first = False

        def piece_done(n):
            m = next_on_slot.get(n)
            if m is not None:
                emit_piece_load(m)

        offs = {"r": 0, "f": 0}

        def take(shape, a):
            work, lim = (workr, WNR) if a == "r" else (workf, WNF)
            ap = carve(work, offs[a], shape)
            offs[a] += int(np.prod(shape[1:]))
            assert offs[a] <= lim, (a, offs[a])
            return ap

        def tR(shape):
            return take(shape, "r")

        def tF(shape):
            return take(shape, "f")
        xtok = tF([128, 2, D])
        ebias = tF([128, 3, 8, 128])
        ln1g = tF([128, D])
        ln1b = tF([128, D])
        xT = tR([128, 8, SLAB])
        attnT = tR([128, 4, TT])
        zsT = tR([128, 4, TT])
        o_r, o_f = offs["r"], offs["f"]
        qT = tR([128, 4, TT])
        kT0 = tR([128, SLAB])
        kT1 = tR([128, SLAB])
        vaug = tR([128, 6, 2, 66])
        PT2 = tR([128, 2, 512])
        yT = tR([128, 4, TT])
        expS2 = tF([128, 2, 512])
        attn_tok = tF([128, 512])
        pT = tF([128, 4, PWN])
        ptmp = tF([128, 2, PWN])
        h1T = tF([128, 8, 128])
        offs["r"], offs["f"] = o_r, o_f
        mT = tR([128, 8, TT])
        sa = tF([128, TT])
        spb = tF([128, TT])
        r1 = tF([128, 3, D])
        ntmp1 = tF([128, D])

        lg = small[:, 0:32]
        m8 = small[:, 32:40]
        mask = small[:, 40:72]
        posb = small[:, 72:104]
        dest = small[:, 104:136]
        ovf = small[:, 136:168]
        junk = small[:, 168:200]
        destf = small[:, 200:204]
        negm = small[:, 204:205]
        ex4 = small[:, 208:212]
        gs = small[:, 212:213]
        rs = small[:, 213:214]
        stats = small[:, 216:228]
        mv = small[:, 228:230]
        rstd = small[:, 230:231]
        nmr = small[:, 231:232]
        den = small[:, 232:240]
        rec = small[:, 240:248]
        eps_t = small[:, 248:249]

        w_in_v = w_in.rearrange("(kc p) n -> p kc n", p=128)
        wout_v = wout.rearrange("(kc p) n -> p kc n", p=128)
        wab_v = wab.rearrange("(kc p) n -> p kc n", p=128)
        wpb_v = wpb.rearrange("(kc p) n -> p kc n", p=128)
        wpg_v = wpg.rearrange("(g c) d -> c g d", c=128)
        wq_v = w_in[:, 0:512].rearrange("(kc p) (t i d) -> p kc i t d", p=128, t=2, i=4)

        def v3(a, b):
            return lambda sl, a=a, b=b: sl[:, 0:a * b].rearrange("p (a b) -> p a b", a=a)

        def v3o(o_, a, b):
            return lambda sl, o_=o_, a=a, b=b: sl[:, o_:o_ + a * b].rearrange("p (a b) -> p a b", a=a)

        def vq(i, t_):
            return lambda sl, i=i, t_=t_: sl[:, 0:4096].rearrange("p (kc i t d) -> p kc i t d", kc=8, i=4, t=2)[:, :, i, t_, :]

        PI = {}
        for t in range(NTILE):
            PI[(t, "kv")] = len(pieces); pieces.append([(v3(8, 256), w_in_v[:, :, 512:768])])
            PI[(t, "q")] = len(pieces)
            pieces.append([(vq(i, t_), wq_v[:, :, i, t_, :]) for i in range(4) for t_ in range(2)])
            PI[(t, "p")] = len(pieces); pieces.append([(v3(8, 512), w_in_v[:, :, 768:1280])])
            PI[(t, "pg")] = len(pieces); pieces.append([(v3(4, 128), wpg_v)])
            for h_ in range(2):
                PI[(t, "br", h_)] = len(pieces)
                pieces.append([(v3o(0, 4, 512), wab_v[:, :, h_ * 512:(h_ + 1) * 512]),
                               (v3o(2048, 4, 512), wpb_v[:, :, h_ * 512:(h_ + 1) * 512])])
                for jj in (2 * h_, 2 * h_ + 1):
                    PI[(t, "g", jj)] = len(pieces)
                    pieces.append([(v3o(0, 8, 256), w_in_v[:, :, 1280 + jj * 256:1280 + (jj + 1) * 256]),
                                   (v3o(2048, 8, 256), w_in_v[:, :, 2304 + jj * 256:2304 + (jj + 1) * 256])])
            for nh in range(2):
                PI[(t, "wo", nh)] = len(pieces)
                pieces.append([(v3(8, 512), wout_v[:, :, nh * 512:(nh + 1) * 512])])
        if full:
            for e in range(NE):
                w1e = w1[e].rearrange("(kc p) (t g c) -> p kc t g c", p=128, t=2, g=2)
                w2e = w2[e].rearrange("(fc p) n -> p fc n", p=128)
                for cg_ in range(2):
                    for kh in range(2):
                        PI[(e, "w1", cg_, kh)] = len(pieces)
                        pieces.append([(lambda sl, t_=t_: sl[:, 0:4096].rearrange("p (kc t c) -> p kc t c", kc=4, t=2)[:, :, t_, :],
                                        w1e[:, kh * 4:(kh + 1) * 4, t_, cg_, :]) for t_ in range(2)])
                for kk in range(2):
                    PI[(e, "w2", kk)] = len(pieces)
                    pieces.append([(v3(4, 1024), w2e[:, kk * 4:(kk + 1) * 4, :])])

        n0_moe = PI[(0, "w1", 0, 0)] if full else len(pieces)
        last = {}
        for n in range(len(pieces)):
            sl_ = n % NRING if n < n0_moe else (n - n0_moe) % NR2
            slot_of[n] = sl_
            if sl_ in last:
                next_on_slot[last[sl_]] = n
            last[sl_] = n

        def pslot(key):
            n = PI[key]
            return n, ring(slot_of[n]), f"ring{slot_of[n]}"

        def ld(dst, src, key, sem=None):
            S.op("sp", I("dma_start", out=dst, in_=src), writes=[key], dma=(sem or "c_" + key))

        ld(ident[:], ident_d, "ident")
        ld(tri[:], tri_d, "tri")
        ld(ones[:], ones_d, "ones")
        ld(ecol[:], ecol_d, "ecol")
        ld(esink[:], sinks_d, "esink")
        ld(pscale[:], pscale_d, "pscale")
        ld(brout[:], brout_d, "brout")
        if full:
            ld(b1T[:], b1T_d, "b1T")
        ld(r3(wr[:], 8), wr_d.rearrange("(kc p) e -> p kc e", p=128), "wr")
        ld(ebias.rearrange("p a b c -> p (a b c)"), ebias_d, "ebias")
        ld(ln1g, ln1g_d, "ln1g")
        ld(ln1b, ln1b_d, "ln1b")
        for n in range(min(NRING, len(pieces))):
            emit_piece_load(n)
        S.op("act", I("activation", out=esink[:], in_=esink[:], func=AF.Exp), reads=["esink"], writes=["esink"])
        S.op("dve", I("memset", runcnt[:], 0.0), writes=["runcnt"])
        S.op("dve", I("memset", eps_t, LN_EPS), writes=["eps"])
        zf = ptmp.rearrange("p a b -> p (a b)")

        def zero_fill():
            S.op("dve", I("memset", zf, 0.0), writes=["ptmp0", "ptmp1"])
            S.op("dve", I("tensor_copy", R(kT0[64:128, :]), zf[64:128, 0:SLAB]), reads=["ptmp0", "ptmp1"], cwrites=["kT"])
            S.op("dve", I("tensor_copy", R(kT1[0:64, :]), zf[0:64, 0:SLAB]), reads=["ptmp0", "ptmp1"], cwrites=["kT"])
            for hd in range(2):
                S.op("dve", I("tensor_copy", R(vaug[:, :, hd, 65]), zf[:, 0:6]), reads=["ptmp0", "ptmp1"], cwrites=["vaug"])

        alt = [0]

        def evac(out_ap, in_ap, reads, writes=(), cwrites=(), eng=None):
            if eng is None:
                eng = "act" if alt[0] % 2 == 0 else "dve"
                alt[0] += 1
            if eng == "act":
                S.op("act", I("copy", out=out_ap, in_=in_ap), reads=reads, writes=writes, cwrites=cwrites)
            else:
                S.op("dve", I("tensor_copy", out_ap, in_ap), reads=reads, writes=writes, cwrites=cwrites)

        def layer_norm_tok(src, gam, bet, dst, ntmp, key_src, key_dst, gkey, bkey, nkey="ntmp"):
            for c in range(2):
                S.op("dve", I("bn_stats", out=stats[:, c * 6:(c + 1) * 6], in_=src[:, c * 512:(c + 1) * 512]),
                     reads=[key_src], cwrites=["stats"])
            S.op("dve", I("bn_aggr", out=mv, in_=stats), reads=["stats"], writes=["mv"])
            S.op("act", I("activation", out=rstd, in_=mv[:, 1:2], func=AF.Ln, bias=eps_t, scale=1.0),
                 reads=["mv", "eps"], writes=["rstd"])
            S.op("act", I("activation", out=rstd, in_=rstd, func=AF.Exp, scale=-0.5), reads=["rstd"], writes=["rstd"])
            S.op("dve", I("tensor_scalar", out=nmr, in0=mv[:, 0:1], scalar1=rstd, scalar2=-1.0,
                          op0=ALU.mult, op1=ALU.mult), reads=["mv", "rstd"], writes=["nmr", "stats"])
            S.op("act", I("activation", out=ntmp, in_=src, func=AF.Identity, bias=nmr, scale=rstd),
                 reads=[key_src, "rstd", "nmr"], writes=[nkey])
            S.op("dve", I("tensor_tensor", out=ntmp, in0=ntmp, in1=gam, op=ALU.mult),
                 reads=[nkey, gkey], writes=[nkey])
            S.op("dve", I("tensor_tensor", out=dst, in0=ntmp, in1=bet, op=ALU.add),
                 reads=[nkey, bkey], writes=[key_dst])

        def transp4(dst_bank, src_aps):
            return G([("transpose", (), dict(out=dst_bank[:, c * 128:(c + 1) * 128], in_=a, identity=ident[:]))
                      for c, a in enumerate(src_aps)])

        def mmgroup(out_ap, pairs):
            n = len(pairs)
            return G([("matmul", (out_ap, l, r), dict(start=(i == 0), stop=(i == n - 1)))
                      for i, (l, r) in enumerate(pairs)])

        wrv = r3(wr[:], 8)

        def router(t, tbs=range(4)):
            for tb in tbs:
                tbg = t * 4 + tb
                rb = xtok[:, tb % 2, :]
                rk = f"xtok{tb % 2}"
                S.op("sp", I("dma_start", out=rb, in_=h1_scr[tbg * 128:(tbg + 1) * 128, :]),
                     reads=["h1_scr"], writes=[rk], dma=rk)
                for hb in range(2):
                    pb_, pk, _ = bank()
                    S.op("pe", transp4(pb_, [rb[:, (hb * 4 + c) * 128:(hb * 4 + c + 1) * 128] for c in range(4)]),
                         reads=[rk, "ident"], writes=[pk])
                    evac(h1T[:, hb * 4:(hb + 1) * 4, :], r3(pb_[:, :], 4), reads=[pk], cwrites=["h1T"])
                pb_, pk, _ = bank()
                S.op("pe", mmgroup(pb_[:, 0:32], [(h1T[:, kc, :], wrv[:, kc, :]) for kc in range(8)]),
                     reads=["h1T", "wr"], writes=[pk])
                S.op("dve", I("tensor_tensor", out=lg, in0=pb_[:, 0:32], in1=brout[:], op=ALU.add),
                     reads=[pk, "brout"], writes=["lg"])
                S.op("dve", I("max", out=m8, in_=lg), reads=["lg"], writes=["m8"])
                S.op("dve", I("tensor_scalar", out=mask, in0=lg, scalar1=m8[:, 3:4], scalar2=None, op0=ALU.is_ge),
                     reads=["lg", "m8"], writes=["mask"])
                pb2, pk2, _ = bank()
                S.op("pe", I("matmul", pb2[:, 0:32], tri[:], mask, start=True, stop=True), reads=["tri", "mask"], writes=[pk2])
                S.op("pe", I("matmul", pb2[:, 32:64], ones[:], mask, start=True, stop=True), reads=["ones", "mask"], cwrites=[pk2])
                S.op("dve", I("tensor_tensor", out=posb, in0=pb2[:, 0:32], in1=runcnt[:], op=ALU.add),
                     reads=[pk2, "runcnt"], writes=["posb"])
                S.op("dve", I("tensor_tensor", out=runcnt[:], in0=runcnt[:], in1=pb2[:, 32:64], op=ALU.add),
                     reads=[pk2, "runcnt"], writes=["runcnt"])
                S.op("dve", I("tensor_scalar", out=ovf, in0=posb, scalar1=float(CAP), scalar2=None, op0=ALU.is_ge),
                     reads=["posb"], writes=["ovf"])
                S.op("dve", I("tensor_tensor", out=dest, in0=posb, in1=ecol[:], op=ALU.add),
                     reads=["posb", "ecol"], writes=["dest"])
                S.op("dve", I("tensor_scalar", out=posb, in0=dest, scalar1=-1.0, scalar2=float(NE * CAP),
                              op0=ALU.mult, op1=ALU.add), reads=["dest"], writes=["posb"])
                S.op("dve", I("tensor_tensor", out=posb, in0=posb, in1=ovf, op=ALU.mult), reads=["posb", "ovf"], writes=["posb"])
                S.op("dve", I("tensor_tensor", out=dest, in0=dest, in1=posb, op=ALU.add), reads=["dest", "posb"], writes=["dest"])
                for k in range(4):
                    S.op("dve", I("scalar_tensor_tensor", out=junk, in0=lg, scalar=m8[:, k:k + 1], in1=dest,
                                  op0=ALU.is_equal, op1=ALU.mult, accum_out=destf[:, k:k + 1]),
                         reads=["lg", "m8", "dest"], writes=["junk"], cwrites=["destf"])
                S.op("dve", I("tensor_copy", idx_all[:, tbg * 4:(tbg + 1) * 4], destf), reads=["destf"], writes=[f"idx{tbg}"])
                S.op("dve", I("tensor_scalar", out=negm, in0=m8[:, 0:1], scalar1=-1.0, scalar2=None, op0=ALU.mult),
                     reads=["m8"], writes=["negm"])
                S.op("act", I("activation", out=ex4, in_=m8[:, 0:4], func=AF.Exp, bias=negm, scale=1.0, accum_out=gs),
                     reads=["m8", "negm"], writes=["ex4", "gs"])
                S.op("dve", I("reciprocal", out=rs, in_=gs), reads=["gs"], writes=["rs"])
                S.op("dve", I("tensor_scalar", out=gates_all[:, tbg * 4:(tbg + 1) * 4], in0=ex4, scalar1=rs,
                              scalar2=None, op0=ALU.mult), reads=["ex4", "rs"], writes=[f"gates{tbg}"])
                for k in range(4 if stage != "h1ns" else 0):
                    S.op("gp", I("indirect_dma_start", out=xs_scr,
                                 out_offset=bass.IndirectOffsetOnAxis(ap=idx_all[:, tbg * 4 + k:tbg * 4 + k + 1], axis=0),
                                 in_=rb, in_offset=None),
                         reads=[rk, f"idx{tbg}"], cwrites=["xs_scr"], dma=f"scat{tb % 2}")


        def T1_blocks(t, blks):
            for blk in blks:
                xb = xtok[:, blk % 2, :]
                xk = f"xtok{blk % 2}"
                S.op("sp", I("dma_start", out=xb, in_=xs[t, blk * 128:(blk + 1) * 128, :]), writes=[xk], dma=xk)
                for hb in range(2):
                    pb_, pk, _ = bank()
                    S.op("pe", transp4(pb_, [xb[:, (hb * 4 + c) * 128:(hb * 4 + c + 1) * 128] for c in range(4)]),
                         reads=[xk, "ident"], writes=[pk])
                    evac(R(xT[:, hb * 4:(hb + 1) * 4, blk * 128:(blk + 1) * 128]), r3(pb_[:, :], 4),
                         reads=[pk], cwrites=[f"xT{blk}"], eng=("act" if t > 0 else None))

        for t in range(NTILE):
            ld(kval[:], kval_d[t], "kval", sem="kval")
            ld(icnt[:], icnt_d[t], "icnt", sem="icnt")
            zero_fill()
            if t == 0:
                T1_blocks(0, range(6))
            xTall = [f"xT{b}" for b in range(6)]
            if STOP <= 1:
                break
            n_kv, s_kv, k_kv = pslot((t, "kv"))
            kvv = r3(s_kv[:, 0:2048], 8)
            for hh in range(2):
                pb_, pk, _ = bank()
                S.op("pe", mmgroup(pb_[:, 0:384], [(kvv[:, kc, 0:128], R(xT[:, kc, hh * 384:(hh + 1) * 384]))
                                                   for kc in range(8)]), reads=[k_kv] + xTall, writes=[pk])
                if SUB >= "b":
                    ee = "act" if (hh == 0 or t > 0) else "dve"
                    evac(R(kT0[0:64, hh * 384:(hh + 1) * 384]), pb_[0:64, 0:384], reads=[pk], cwrites=["kT"], eng=ee)
                    evac(R(kT1[64:128, hh * 384:(hh + 1) * 384]), pb_[64:128, 0:384], reads=[pk], cwrites=["kT"], eng=ee)
            for half in range(2 if SUB >= "c" else 0):
                pb_, pk, _ = bank()
                calls = []
                for b3 in range(3):
                    blk = half * 3 + b3
                    for kc in range(8):
                        calls.append(("matmul", (pb_[:, b3 * 128:(b3 + 1) * 128], R(xT[:, kc, blk * 128:(blk + 1) * 128]),
                                                 kvv[:, kc, 128:256]), dict(start=(kc == 0), stop=(kc == 7))))
                S.op("pe", G(calls), reads=[k_kv] + xTall, writes=[pk])
                for b3 in range(3 if SUB >= "d" else 0):
                    blk = half * 3 + b3
                    evac(R(vaug[:, blk, :, 0:64]), r3(pb_[:, b3 * 128:(b3 + 1) * 128], 2), reads=[pk], cwrites=["vaug"],
                         eng=("act" if (half == 0 or t > 0) else "dve"))
            piece_done(n_kv)
            for hd in range(2 if SUB >= "e" else 0):
                S.op("dve", I("tensor_copy", R(vaug[:, :, hd, 64]), kval[:, :]), reads=["kval"], cwrites=["vaug"])
            if STOP <= 2 or (STOP == 3 and SUB != ''):
                break
            n_q, s_q, k_q = pslot((t, "q"))
            qv = r3(s_q[:, 0:4096], 8)
            for i4 in range(4):
                pb_, pk, _ = bank()
                S.op("pe", mmgroup(pb_[:, :], [(qv[:, kc, i4 * 128:(i4 + 1) * 128], R(xT[:, kc, 128:640]))
                                               for kc in range(8)]), reads=[k_q] + xTall, writes=[pk])
                evac(R(qT[:, i4, :]), pb_[:, :], reads=[pk], cwrites=["qT"], eng=("act" if t > 0 else None))
            piece_done(n_q)
            if STOP <= 3:
                break
            n_p, s_p, k_p = pslot((t, "p"))
            pv_ = r3(s_p[:, 0:4096], 8)
            for g in range(4):
                for hh in range(2):
                    pb_, pk, _ = bank()
                    S.op("pe", mmgroup(pb_[:, 0:272], [(pv_[:, kc, g * 128:(g + 1) * 128],
                                                        R(xT[:, kc, PW0 + hh * 272:PW0 + (hh + 1) * 272]))
                                                       for kc in range(8)]), reads=[k_p] + xTall, writes=[pk])
                    evac(pT[:, g, hh * 272:(hh + 1) * 272], pb_[:, 0:272], reads=[pk], cwrites=[f"pT{g}"],
                         eng=("act" if t > 0 else None))
            piece_done(n_p)
            if t > 0:
                router(t - 1)
            if not full and t == 0:
                S.op("sp", I("dma_start", out=dbg_qT, in_=qT.rearrange("p a b -> p (a b)")), reads=["qT"], dma="dbg")
                S.op("sp", I("dma_start", out=dbg_kT[:, 0:768], in_=kT0), reads=["kT"], dma="dbg")
                S.op("sp", I("dma_start", out=dbg_kT[:, 768:1536], in_=kT1), reads=["kT"], dma="dbg")
                S.op("sp", I("dma_start", out=dbg_v, in_=vaug.rearrange("p a b c -> p (a b c)")), reads=["vaug"], dma="dbg")
            if STOP <= 5:
                break
            pool_ops = []

            def PQ(eng, fn, **kw):
                pool_ops.append((eng, fn, kw))

            def pool_drip(n):
                for _ in range(n):
                    if pool_ops:
                        e_, f_, kw_ = pool_ops.pop(0)
                        S.op(e_, f_, **kw_)

            for g in range(4):
                cur = pT[:, g, :]
                curk = f"pT{g}"
                lo, hi = 8, PWN - 8
                for l in range(1, g + 2):
                    dst = ptmp[:, l % 2, :]
                    dk = f"ptmp{l % 2}"
                    if l == 1:
                        PQ("dve", I("tensor_tensor", out=dst[:, lo:hi], in0=cur[:, lo - 1:hi - 1], in1=cur[:, lo:hi],
                                      op=ALU.add), reads=[curk], writes=[dk])
                    else:
                        sh = 2 ** (l - 2)
                        PQ("dve", I("tensor_tensor", out=dst[:, lo:hi], in0=cur[:, lo - sh:hi - sh],
                                      in1=cur[:, lo + sh:hi + sh], op=ALU.add), reads=[curk], writes=[dk])
                    cur, curk = dst, dk
                w = 2 ** (g + 1)
                PQ("dve", I("scalar_tensor_tensor", out=R(yT[:, g, :]), in0=cur[:, 16:528], scalar=1.0 / w,
                              in1=pT[:, g, 16:528], op0=ALU.mult, op1=ALU.subtract),
                     reads=[curk, f"pT{g}"], writes=[f"yT{g}"])
                for ed in range(2):
                    u0 = 16 if ed == 0 else 520
                    y0 = 0 if ed == 0 else 504
                    ic = icnt[:, g * 16 + ed * 8:g * 16 + ed * 8 + 8]
                    PQ("dve", I("tensor_tensor", out=junk[:, 0:8], in0=cur[:, u0:u0 + 8], in1=ic, op=ALU.mult),
                         reads=[curk, "icnt"], writes=["junk"])
                    PQ("dve", I("tensor_tensor", out=R(yT[:, g, y0:y0 + 8]), in0=junk[:, 0:8], in1=pT[:, g, u0:u0 + 8],
                                  op=ALU.subtract), reads=["junk", f"pT{g}"], cwrites=[f"yT{g}"])
            if STOP <= 4:
                break
            pvbs = {}

            def att_S(qb, g, jj):
                if qb not in pvbs:
                    pvbs[qb] = [bank(), bank()]
                    reserved.update([pvbs[qb][0][2], pvbs[qb][1][2]])
                km = kT0 if g == 0 else kT1
                kb = qb + jj
                pb_, pk, _ = bank()
                S.op("pe", I("matmul", pb_[:, :], R(km[:, kb * 128:(kb + 1) * 128]), R(qT[:, :, qb * 128:(qb + 1) * 128]),
                             start=True, stop=True), reads=["kT", "qT"], writes=[pk])
                pp = (qb * 6 + g * 3 + jj) % 2
                S.op("act", I("activation", out=expS2[:, pp, :], in_=pb_[:, :], func=AF.Exp, scale=0.125),
                     reads=[pk], writes=[f"expS{pp}"])
                S.op("dve", I("tensor_tensor", out=r3(R(PT2[:, pp, :]), 4), in0=r3(expS2[:, pp, :], 4),
                              in1=ebias[:, jj, 4 * g:4 * g + 4, :], op=ALU.mult), reads=[f"expS{pp}", "ebias"], writes=[f"PT{pp}"])

            def att_PV(qb, g, jj):
                pp = (qb * 6 + g * 3 + jj) % 2
                PT = PT2[:, pp, :]
                pvt, pvk, _ = pvbs[qb][g]
                calls = [("matmul", (pvt[:, i * 66:(i + 1) * 66], R(PT[:, i * 128:(i + 1) * 128]), R(vaug[:, qb + jj, g, :])),
                          dict(start=(jj == 0 and i == 0), stop=(jj == 2 and i == 3))) for i in range(4)]
                if jj == 0:
                    S.op("pe", G(calls), reads=[f"PT{pp}", "vaug"], writes=[pvk])
                else:
                    S.op("pe", G(calls), reads=[f"PT{pp}", "vaug"], cwrites=[pvk])

            def att_N(qb):
                pvb = pvbs[qb]
                for hb in range(2):
                    pvt, pvk, _ = pvb[hb]
                    S.op("dve", I("tensor_tensor", out=den[:, hb * 4:(hb + 1) * 4], in0=r3(pvt[:, 0:264], 4)[:, :, 64],
                                  in1=esink[:, hb * 4:(hb + 1) * 4], op=ALU.add), reads=[pvk, "esink"], cwrites=["den"])
                S.op("dve", I("reciprocal", out=rec, in_=den), reads=["den"], writes=["rec"])
                for hd in range(8):
                    pvt, pvk, _ = pvb[hd // 4]
                    c0 = (hd % 4) * 66
                    if hd // 4 == 0:
                        S.op("act", I("activation", out=attn_tok[:, hd * 64:(hd + 1) * 64], in_=pvt[:, c0:c0 + 64],
                                      func=AF.Identity, scale=rec[:, hd:hd + 1]), reads=[pvk, "rec"], cwrites=["attn_tok"])
                    else:
                        S.op("dve", I("tensor_scalar", out=attn_tok[:, hd * 64:(hd + 1) * 64], in0=pvt[:, c0:c0 + 64],
                                      scalar1=rec[:, hd:hd + 1], scalar2=None, op0=ALU.mult),
                             reads=[pvk, "rec"], cwrites=["attn_tok"])
                reserved.discard(pvb[0][2])
                reserved.discard(pvb[1][2])

            def att_T(qb):
                pb_, pk, _ = bank()
                S.op("pe", transp4(pb_, [attn_tok[:, c * 128:(c + 1) * 128] for c in range(4)]),
                     reads=["attn_tok", "ident"], writes=[pk])
                evac(R(attnT[:, :, qb * 128:(qb + 1) * 128]), r3(pb_[:, :], 4), reads=[pk], cwrites=["attnT"])

            steps = [(qb, g, jj) for qb in range(4) for g in range(2) for jj in range(3)]
            att_S(*steps[0])
            for si, (qb, g, jj) in enumerate(steps):
                if si + 1 < len(steps):
                    att_S(*steps[si + 1])
                att_PV(qb, g, jj)
                pool_drip(2)
                if g == 0 and jj == 1 and qb > 0:
                    att_T(qb - 1)
                if g == 1 and jj == 2:
                    att_N(qb)
            att_T(3)
            pool_drip(1000)
            if not full and t == 0:
                S.op("sp", I("dma_start", out=dbg_yT, in_=yT.rearrange("p a b -> p (a b)")), reads=[f"yT{g_}" for g_ in range(4)], dma="dbg")
            if STOP <= 6:
                break
            n_pg, s_pg, k_pg = pslot((t, "pg"))
            pgv = r3(s_pg[:, 0:512], 4)
            for g in range(4):
                pb_, pk, _ = bank()
                S.op("pe", I("matmul", pb_[:, :], pgv[:, g, :], R(yT[:, g, :]), start=True, stop=True),
                     reads=[k_pg, f"yT{g}"], writes=[pk])
                S.op("act", I("activation", out=R(zsT[:, g, :]), in_=pb_[:, :], func=AF.Identity, scale=pscale[:, g:g + 1]),
                     reads=[pk, "pscale"], cwrites=["zsT"])
            piece_done(n_pg)
            if STOP <= 7:
                break
            for j in range(8):
                n_g, s_g, k_g = pslot((t, "g", j // 2))
                n_b, s_b, k_b = pslot((t, "br", j // 4))
                cb = (j % 4) * 128
                cg = (j % 2) * 128
                abv = r3(s_b[:, 0:2048], 4)[:, :, cb:cb + 128]
                pbv = r3(s_b[:, 2048:4096], 4)[:, :, cb:cb + 128]
                gav = r3(s_g[:, 0:2048], 8)[:, :, cg:cg + 128]
                gpv = r3(s_g[:, 2048:4096], 8)[:, :, cg:cg + 128]
                bA, kA, _ = bank(); bB, kB, _ = bank(); bC, kC, _ = bank(); bD, kD, _ = bank()
                S.op("pe", mmgroup(bC[:, :], [(gav[:, kc, :], R(xT[:, kc, 128:640])) for kc in range(8)]),
                     reads=[k_g] + xTall, writes=[kC])
                S.op("pe", mmgroup(bD[:, :], [(gpv[:, kc, :], R(xT[:, kc, 128:640])) for kc in range(8)]),
                     reads=[k_g] + xTall, writes=[kD])
                S.op("pe", mmgroup(bA[:, :], [(abv[:, c, :], R(attnT[:, c, :])) for c in range(4)]),
                     reads=[k_b, "attnT"], writes=[kA])
                S.op("pe", mmgroup(bB[:, :], [(pbv[:, c, :], R(zsT[:, c, :])) for c in range(4)]),
                     reads=[k_b, "zsT"], writes=[kB])
                if j % 2 == 1:
                    piece_done(n_g)
                if j % 4 == 3:
                    piece_done(n_b)
                S.op("act", I("activation", out=sa, in_=bC[:, :], func=AF.Sigmoid), reads=[kC], writes=["sa"])
                S.op("act", I("activation", out=spb, in_=bD[:, :], func=AF.Sigmoid), reads=[kD], writes=["spb"])
                S.op("dve", I("tensor_tensor", out=sa, in0=sa, in1=bA[:, :], op=ALU.mult), reads=["sa", kA], writes=["sa"])
                S.op("dve", I("tensor_tensor", out=spb, in0=spb, in1=bB[:, :], op=ALU.mult), reads=["spb", kB], writes=["spb"])
                S.op("dve", I("tensor_tensor", out=R(mT[:, j, :]), in0=sa, in1=spb, op=ALU.add),
                     reads=["sa", "spb"], cwrites=["mT"])
            if not full and t == 0:
                S.op("sp", I("dma_start", out=dbg_attnT, in_=attnT.rearrange("p a b -> p (a b)")), reads=["attnT"], dma="dbg")
                S.op("sp", I("dma_start", out=dbg_zsT, in_=zsT.rearrange("p a b -> p (a b)")), reads=["zsT"], dma="dbg")
                S.op("sp", I("dma_start", out=dbg_mT, in_=mT.rearrange("p a b -> p (a b)")), reads=["mT"], dma="dbg")
            if STOP <= 8:
                break
            n_w0, s_w0, k_w0 = pslot((t, "wo", 0))
            n_w1, s_w1, k_w1 = pslot((t, "wo", 1))
            wov = [r3(s_w0[:, 0:4096], 8), r3(s_w1[:, 0:4096], 8)]
            wok = [k_w0, k_w1]
            def stageA(tb):
                rb = r1[:, tb % 3, :]
                rk = f"r1_{tb % 3}"
                S.op("gp", I("dma_start", out=rb, in_=xs[t, 128 + tb * 128:128 + (tb + 1) * 128, :]),
                     writes=[rk], dma=rk + "ld")
                for nh in range(2):
                    pb_, pk, _ = bank()
                    S.op("pe", mmgroup(pb_[:, :], [(R(mT[:, j, tb * 128:(tb + 1) * 128]), wov[nh][:, j, :]) for j in range(8)]),
                         reads=[wok[nh], "mT"], writes=[pk])
                    S.op("dve", I("scalar_tensor_tensor", out=rb[:, nh * 512:(nh + 1) * 512], in0=rb[:, nh * 512:(nh + 1) * 512],
                                  scalar=ALPHA, in1=pb_[:, :], op0=ALU.mult, op1=ALU.add), reads=[pk, rk], cwrites=[rk])
                if tb == 3:
                    piece_done(n_w0)
                    piece_done(n_w1)

            stageA(0)
            for tb in range(4):
                tbg = t * 4 + tb
                rb = r1[:, tb % 3, :]
                rk = f"r1_{tb % 3}"
                if tb + 1 < 4:
                    stageA(tb + 1)
                layer_norm_tok(rb, ln1g, ln1b, rb, ntmp1, rk, rk, "ln1g", "ln1b")
                S.op("gp", I("dma_start", out=h1_scr[tbg * 128:(tbg + 1) * 128, :], in_=rb),
                     reads=[rk], cwrites=["h1_scr"], dma=rk + "st")
                if t + 1 < NTILE and STOP == 99:
                    T1_blocks(t + 1, {0: [0, 1], 1: [2, 3], 2: [4], 3: [5]}[tb])
                if t == NTILE - 1:
                    router(t, [tb])

        if not full and STOP == 99:
            S.op("sp", I("dma_start", out=dbg_idx, in_=idx_all[:]), reads=[f"idx{i}" for i in range(16)], dma="dbg")
            S.op("sp", I("dma_start", out=dbg_gates, in_=gates_all[:]), reads=[f"gates{i}" for i in range(16)], dma="dbg")

        if full:
            S.barrier()
            offs["r"], offs["f"] = 0, 0
            xs_tok = tF([128, 3, D])
            xsT = tR([128, 8, CAP])
            actT = tR([128, 8, CAP])
            gbuf = tF([128, 2, CAP])
            sgb = tF([128, 2, CAP])
            ubuf = tF([128, 2, CAP])
            y_tok = tF([128, 3, D])
            b2bc = tF([128, 2, D])
            E0 = tR([128, 128])
            b2pad = tR([128, 2, D])
            assert offs["r"] <= XOFF

            def load_xs(e):
                S.op("sp", I("dma_start", out=xs_tok,
                             in_=xs_scr[e * CAP:(e + 1) * CAP, :].rearrange("(b p) d -> p b d", p=128)),
                     reads=["xs_scr"], writes=["xs_tok"], dma="xs_tok")

            def transposes(e):
                for kc in range(8):
                    pb_, pk, _ = bank()
                    S.op("pe", G([("transpose", (), dict(out=pb_[:, b * 128:(b + 1) * 128],
                                                         in_=xs_tok[:, b, kc * 128:(kc + 1) * 128], identity=ident[:]))
                                  for b in range(3)]), reads=["xs_tok", "ident"], writes=[pk])
                    evac(R(xsT[:, kc, :]), pb_[:, 0:CAP], reads=[pk], cwrites=["xsT"], eng="act")

            def mlp1(e):
                S.op("gp", I("dma_start", out=R(b2pad[0:1, e % 2, :]), in_=b2_d[e:e + 1, :]),
                     writes=[f"b2pad{e % 2}"], dma=f"b2pad{e % 2}")
                for cg_ in range(2):
                    n_a, s_a, k_a = pslot((e, "w1", cg_, 0))
                    n_b, s_b, k_b = pslot((e, "w1", cg_, 1))
                    wh = [s_a[:, 0:4096].rearrange("p (kc t c) -> p kc t c", kc=4, t=2),
                          s_b[:, 0:4096].rearrange("p (kc t c) -> p kc t c", kc=4, t=2)]
                    for ii in range(4):
                        i8 = cg_ * 4 + ii
                        bG, kG, _ = bank(); bU, kU, _ = bank()
                        S.op("pe", mmgroup(bG[:, 0:CAP], [(wh[kc // 4][:, kc % 4, 0, ii * 128:(ii + 1) * 128], R(xsT[:, kc, :]))
                                                          for kc in range(8)]), reads=[k_a, k_b, "xsT"], writes=[kG])
                        S.op("pe", mmgroup(bU[:, 0:CAP], [(wh[kc // 4][:, kc % 4, 1, ii * 128:(ii + 1) * 128], R(xsT[:, kc, :]))
                                                          for kc in range(8)]), reads=[k_a, k_b, "xsT"], writes=[kU])
                        p2 = i8 % 2
                        gb = gbuf[:, p2, :]; sg = sgb[:, p2, :]; ub = ubuf[:, p2, :]
                        cg = e * 16 + i8
                        cu = e * 16 + 8 + i8
                        S.op("dve", I("tensor_scalar", out=gb, in0=bG[:, 0:CAP], scalar1=b1T[:, cg:cg + 1], scalar2=7.0,
                                      op0=ALU.add, op1=ALU.min), reads=[kG, "b1T"], writes=[f"gb{p2}"])
                        S.op("act", I("activation", out=sg, in_=gb, func=AF.Sigmoid, scale=1.702),
                             reads=[f"gb{p2}"], writes=[f"sg{p2}"])
                        S.op("dve", I("tensor_scalar", out=ub, in0=bU[:, 0:CAP], scalar1=b1T[:, cu:cu + 1], scalar2=7.0,
                                      op0=ALU.add, op1=ALU.min), reads=[kU, "b1T"], writes=[f"ub{p2}"])
                        S.op("dve", I("tensor_scalar", out=ub, in0=ub, scalar1=-7.0, scalar2=1.0, op0=ALU.max, op1=ALU.add),
                             reads=[f"ub{p2}"], writes=[f"ub{p2}"])
                        S.op("dve", I("tensor_tensor", out=gb, in0=gb, in1=sg, op=ALU.mult),
                             reads=[f"gb{p2}", f"sg{p2}"], writes=[f"gb{p2}"])
                        S.op("dve", I("tensor_tensor", out=R(actT[:, i8, :]), in0=ub, in1=gb, op=ALU.mult),
                             reads=[f"gb{p2}", f"ub{p2}"], cwrites=["actT"])
                    piece_done(n_a)
                    piece_done(n_b)

            def mlp2(e):
                n_a, s_a, k_a = pslot((e, "w2", 0))
                n_b, s_b, k_b = pslot((e, "w2", 1))
                w2v = [r3(s_a[:, 0:4096], 4), r3(s_b[:, 0:4096], 4)]
                bb = b2bc[:, e % 2, :]
                bk = f"b2bc{e % 2}"
                for nh in range(2):
                    pbb, pkb, _ = bank()
                    S.op("pe", I("matmul", pbb[:, :], R(E0[:, :]), R(b2pad[:, e % 2, nh * 512:(nh + 1) * 512]), start=True, stop=True),
                         reads=["E0", f"b2pad{e % 2}"], writes=[pkb])
                    kwb = dict(writes=[bk]) if nh == 0 else dict(cwrites=[bk])
                    S.op("act", I("copy", out=bb[:, nh * 512:(nh + 1) * 512], in_=pbb[:, :]), reads=[pkb], **kwb)
                for b in range(3):
                    yb = y_tok[:, b, :]
                    yk = f"ytok{b}"
                    for nh in range(2):
                        pb_, pk, _ = bank()
                        S.op("pe", mmgroup(pb_[:, :], [(R(actT[:, fc, b * 128:(b + 1) * 128]),
                                                        w2v[fc // 4][:, fc % 4, nh * 512:(nh + 1) * 512]) for fc in range(8)]),
                             reads=[k_a, k_b, "actT"], writes=[pk])
                        kw = dict(writes=[yk]) if nh == 0 else dict(cwrites=[yk])
                        S.op("dve", I("tensor_tensor", out=yb[:, nh * 512:(nh + 1) * 512], in0=pb_[:, :],
                                      in1=bb[:, nh * 512:(nh + 1) * 512], op=ALU.add), reads=[pk, bk], **kw)
                    S.op("sp", I("dma_start", out=ys_scr[e * CAP + b * 128:e * CAP + (b + 1) * 128, :], in_=yb),
                         reads=[yk], cwrites=["ys_scr"], dma=yk)
                piece_done(n_a)
                piece_done(n_b)

            for n in range(n0_moe, len(pieces)):
                if slot_of[n] >= NRING and (n - n0_moe) < NR2:
                    emit_piece_load(n)
            S.op("dve", I("memset", y_tok[:, 0, :], 0.0), writes=["ytok0"])
            S.op("dve", I("memset", y_tok[:, 1, :], 0.0), writes=["ytok1"])
            for i_ in range(2):
                S.op("dve", I("tensor_copy", R(b2pad[:, i_, :]), y_tok[:, i_, :]), reads=[f"ytok{i_}"], writes=[f"b2pad{i_}"])
            S.op("dve", I("tensor_copy", y_tok[0:1, 1, 0:128], ones[0:1, :]), reads=["ones"], writes=["ytok1"])
            S.op("dve", I("tensor_copy", R(E0[:, :]), y_tok[:, 1, 0:128]), reads=["ytok1"], writes=["E0"])
            S.op("sp", I("dma_start", out=ys_scr[NE * CAP:NE * CAP + 128, :], in_=y_tok[:, 0, :]),
                 reads=["ytok0"], cwrites=["ys_scr"], dma="ytok0")
            load_xs(0)
            transposes(0)
            for e in range(NE):
                mlp1(e)
                if e + 1 < NE:
                    load_xs(e + 1)
                    transposes(e + 1)
                mlp2(e)

            S.barrier()
            offs["r"], offs["f"] = 0, 0
            yg = tR([128, 2, 4, D])
            dg = tR([128, 2, 4, 128])
            ln2g = tF([128, D])
            ln2b = tF([128, D])
            hb2 = tF([128, 2, D])
            ob2 = tF([128, 2, D])
            ld(ln2g, ln2g_d, "ln2g")
            ld(ln2b, ln2b_d, "ln2b")

            def gathers(tbg):
                p2 = tbg % 2
                for k in range(4):
                    kw = dict(writes=[f"yg{p2}"]) if k == 0 else dict(cwrites=[f"yg{p2}"])
                    S.op("gp", I("indirect_dma_start", out=R(yg[:, p2, k, :]), out_offset=None, in_=ys_scr,
                                 in_offset=bass.IndirectOffsetOnAxis(ap=idx_all[:, tbg * 4 + k:tbg * 4 + k + 1], axis=0)),
                         reads=["ys_scr", f"idx{tbg}"], dma=f"yg{p2}", **kw)
                S.op("sp", I("dma_start", out=hb2[:, p2, :], in_=h1_scr[tbg * 128:(tbg + 1) * 128, :]),
                     reads=["h1_scr"], writes=[f"hb{p2}"], dma=f"hb{p2}")

            def combine_mm(tbg):
                p2 = tbg % 2
                for k in range(4):
                    kw = dict(writes=[f"dg{p2}"]) if k == 0 else dict(cwrites=[f"dg{p2}"])
                    S.op("dve", I("tensor_scalar", out=R(dg[:, p2, k, :]), in0=ident[:],
                                  scalar1=gates_all[:, tbg * 4 + k:tbg * 4 + k + 1], scalar2=None, op0=ALU.mult),
                         reads=["ident", f"gates{tbg}"], **kw)
                res = []
                for nh in range(2):
                    pb_, pk, bi = bank()
                    reserved.add(bi)
                    S.op("pe", mmgroup(pb_[:, :], [(R(dg[:, p2, k, :]), R(yg[:, p2, k, nh * 512:(nh + 1) * 512])) for k in range(4)]),
                         reads=[f"dg{p2}", f"yg{p2}"], writes=[pk])
                    res.append((pb_, pk, bi))
                return res

            gathers(0)
            gathers(1)
            pend = combine_mm(0)
            for tbg in range(16):
                p2 = tbg % 2
                cur = pend
                if tbg + 1 < 16:
                    pend = combine_mm(tbg + 1)
                hb = hb2[:, p2, :]
                hk = f"hb{p2}"
                for nh in range(2):
                    pb_, pk, bi = cur[nh]
                    S.op("dve", I("scalar_tensor_tensor", out=hb[:, nh * 512:(nh + 1) * 512], in0=hb[:, nh * 512:(nh + 1) * 512],
                                  scalar=ALPHA, in1=pb_[:, :], op0=ALU.mult, op1=ALU.add), reads=[pk, hk], cwrites=[hk])
                    reserved.discard(bi)
                ob = ob2[:, p2, :]
                ok_ = f"ob{p2}"
                layer_norm_tok(hb, ln2g, ln2b, ob, ob, hk, ok_, "ln2g", "ln2b", nkey=ok_)
                if tbg + 2 < 16:
                    gathers(tbg + 2)
                S.op("sp", I("dma_start", out=out_d[tbg * 128:(tbg + 1) * 128, :], in_=ob),
                     reads=[ok_], cwrites=["out"], dma=f"outst{p2}")

        S.final_waits("sp")

        @block.sync
        def _(h):
            S.replay("sp", h)

        @block.tensor
        def _(h):
            S.replay("pe", h)

        @block.scalar
        def _(h):
            S.replay("act", h)

        @block.vector
        def _(h):
            S.replay("dve", h)

        @block.gpsimd
        def _(h):
            S.replay("gp", h)
    return nc


def _consts():
    slopes = np.asarray([2.0 ** (-8.0 * (h + 1) / 8) for h in range(8)], np.float64)
    kk = np.arange(128)[:, None]
    qq = np.arange(128)[None, :]
    eb = np.zeros((128, 3, 8, 128), np.float64)
    for jj in range(3):
        rel = kk + (jj - 1) * 128 - qq
        valid = np.abs(rel) <= 128
        for h in range(8):
            eb[:, jj, h, :] = np.where(valid, np.exp(-slopes[h] * np.abs(rel)), 0.0)
    ident = np.eye(128, dtype=np.float32)
    tri = (np.arange(128)[:, None] < np.arange(128)[None, :]).astype(np.float32)
    ones = np.ones((128, 128), np.float32)
    ecol = np.broadcast_to((np.arange(NE) * CAP).astype(np.float32)[None, :], (128, NE)).copy()
    return dict(ebias=eb.reshape(128, 3072).astype(np.float32), ident=ident, tri=tri, ones=ones, ecol=ecol)


def _bc(v):
    return np.ascontiguousarray(np.broadcast_to(np.asarray(v, np.float32).reshape(1, -1), (128, v.size)))


def make_in_maps(inputs, stage="full"):
    x = np.asarray(inputs["x"], np.float32)
    cst = _consts()
    shared = dict(cst)
    shared["sinks"] = _bc(inputs["attn_sinks"][0])
    shared["ln1g"] = _bc(inputs["ln1_g"][0]); shared["ln1b"] = _bc(inputs["ln1_b"][0])
    shared["ln2g"] = _bc(inputs["ln2_g"][0]); shared["ln2b"] = _bc(inputs["ln2_b"][0])
    shared["pscale"] = np.ascontiguousarray(np.asarray(inputs["pool_scale"][0], np.float32).reshape(4, 128).T)
    shared["brout"] = _bc(inputs["b_router"][0])
    shared["b1T"] = np.ascontiguousarray(
        np.asarray(inputs["b_mlp1"][0], np.float32).reshape(NE, 16, 128).transpose(2, 0, 1).reshape(128, NE * 16))
    shared["b2"] = np.ascontiguousarray(np.asarray(inputs["b_mlp2"][0], np.float32))
    shared["wr"] = np.ascontiguousarray(np.asarray(inputs["w_router"][0], np.float32))
    shared["w_in"] = np.ascontiguousarray(np.asarray(inputs["w_in"][0], np.float32))
    shared["wab"] = np.ascontiguousarray(np.asarray(inputs["w_attn_branch"][0], np.float32))
    shared["wpg"] = np.ascontiguousarray(np.asarray(inputs["w_pool_group"][0], np.float32).reshape(512, 128))
    shared["wpb"] = np.ascontiguousarray(np.asarray(inputs["w_pool_branch"][0], np.float32))
    shared["wout"] = np.ascontiguousarray(np.asarray(inputs["w_out"][0], np.float32))
    shared["w1"] = np.ascontiguousarray(np.asarray(inputs["w_mlp1"][0], np.float32))
    shared["w2"] = np.ascontiguousarray(np.asarray(inputs["w_mlp2"][0], np.float32))
    maps = []
    for c in range(NCORES):
        b = c // 4
        s0 = (c % 4) * TOK
        xs = np.zeros((NTILE, SLAB, D), np.float32)
        kval = np.zeros((NTILE, 128, 6), np.float32)
        icnt = np.zeros((NTILE, 128, 64), np.float32)
        for t in range(NTILE):
            st = s0 + t * TT - 128
            lo, hi = max(st, 0), min(st + SLAB, SEQ)
            xs[t, lo - st:hi - st] = x[b, lo:hi]
            pos = st + np.arange(SLAB)
            v = ((pos >= 0) & (pos < SEQ)).astype(np.float32)
            kval[t] = v.reshape(6, 128).T
            for g, w in enumerate((2, 4, 8, 16)):
                for ed in range(2):
                    tt = s0 + t * TT + (np.arange(8) if ed == 0 else 504 + np.arange(8))
                    lo_ = np.maximum(tt - w // 2, 0)
                    hi_ = np.minimum(tt + w // 2 - 1, SEQ - 1)
                    icnt[t, :, g * 16 + ed * 8:g * 16 + ed * 8 + 8] = (1.0 / (hi_ - lo_ + 1)).astype(np.float32)[None, :]
        m = dict(shared)
        m["xs"] = xs; m["kval"] = kval; m["icnt"] = icnt
        if stage != "full":
            for k in ("ln2g", "ln2b", "b1T", "b2", "w1", "w2"):
                m.pop(k)
        maps.append(m)
    return maps


def kernel(**inputs):
    nc = build_program("full")
    maps = make_in_maps(inputs)
    res = run_bass_kernel_spmd(nc, maps, core_ids=list(range(NCORES)))
    outs = [np.asarray(r["out"], np.float32) for r in res.results]
    full = np.concatenate(outs, axis=0).reshape(2, SEQ, D)
    return full
```

```python
import numpy as np
import concourse.bass as bass
import concourse.mybir as mybir
from concourse.bass_utils import run_bass_kernel_spmd

F32 = mybir.dt.float32
F32R = mybir.dt.float32r
I32 = mybir.dt.int32
AF = mybir.ActivationFunctionType
ALU = mybir.AluOpType
AX = mybir.AxisListType

NCORES = 8
D = 1024
SEQ = 8192
TOK = 2048
TT = 512
NTILE = 4
SLAB = 768
NE = 32
CAP = 384
PW0 = 112
PWN = 544
ALPHA = 2.0 ** 0.25
LN_EPS = 1e-5
RSLOT = 4096
NRING = 5


class Sched:
    def __init__(self, sems):
        self.pool_sems = list(sems)
        self.eng = {}
        for n in ("pe", "act", "dve", "gp", "sp"):
            self.eng[n] = dict(sem=self.pool_sems.pop(), count=0, prog=[], seen={}, pend={})
        self.dsem = {}
        self.buf = {}
        self.fence = {}

    def _b(self, k):
        if k not in self.buf:
            self.buf[k] = dict(w={}, r={})
        return self.buf[k]

    @staticmethod
    def _mx(d, tok):
        s, v = tok
        key = id(s)
        if key not in d or d[key][1] < v:
            d[key] = (s, v)

    def op(self, eng, fn, reads=(), writes=(), cwrites=(), dma=None):
        E = self.eng[eng]
        fr, fw = set(), set()
        for k in list(reads) + list(writes) + list(cwrites):
            if k in self.fence:
                fk, mode = self.fence[k]
                (fr if mode == "X" else fw).add(fk)
        if fr or fw:
            reads = list(reads) + sorted(fr)
            cwrites = list(cwrites) + sorted(fw)
        deps = {}
        raw = {}
        for k in reads:
            for t in self._b(k)["w"].values():
                self._mx(deps, t)
                self._mx(raw, t)
        for k in writes:
            b = self._b(k)
            for t in b["w"].values():
                self._mx(deps, t)
            for t in b["r"].values():
                self._mx(deps, t)
        for k in cwrites:
            for t in self._b(k)["r"].values():
                self._mx(deps, t)
        for t in E["pend"].values():
            self._mx(deps, t)
        E["pend"] = {}
        waits = []
        for key, (s, v) in deps.items():
            if s is E["sem"]:
                if eng == "pe" or key not in raw:
                    continue
                v = raw[key][1]
            if E["seen"].get(key, 0) >= v:
                continue
            E["seen"][key] = v
            waits.append((s, v))
        if dma is None:
            E["count"] += 1
            tok = (E["sem"], E["count"])
            inc = (E["sem"], 1)
        else:
            if dma not in self.dsem:
                self.dsem[dma] = [self.pool_sems.pop(), 0]
            ds = self.dsem[dma]
            ds[1] += 16
            tok = (ds[0], ds[1])
            inc = (ds[0], 16)
        E["prog"].append((waits, fn, inc))
        for k in reads:
            self._mx(self._b(k)["r"], tok)
        for k in writes:
            b = self._b(k)
            b["w"] = {}
            b["r"] = {}
            self._mx(b["w"], tok)
        for k in cwrites:
            self._mx(self._b(k)["w"], tok)
        return tok

    def barrier(self):
        toks = [(e["sem"], e["count"]) for e in self.eng.values() if e["count"] > 0]
        toks += [(d[0], d[1]) for d in self.dsem.values() if d[1] > 0]
        for e in self.eng.values():
            for t in toks:
                if t[0] is not e["sem"]:
                    self._mx(e["pend"], t)

    def final_waits(self, eng):
        E = self.eng[eng]
        toks = [(e["sem"], e["count"]) for n, e in self.eng.items() if e["count"] > 0 and n != eng]
        toks += [(d[0], d[1]) for d in self.dsem.values() if d[1] > 0]
        E["final"] = toks

    def replay(self, eng, h):
        E = self.eng[eng]
        for waits, fn, (s, n) in E["prog"]:
            for (ws, wv) in waits:
                h.wait_ge(ws, wv)
            inst = fn(h)
            inst.then_inc(s, n)
        for (ws, wv) in E.get("final", []):
            h.wait_ge(ws, wv)


def I(m, *a, **k):
    return lambda h: getattr(h, m)(*a, **k)


def G(calls):
    def f(h):
        r = None
        for (m, a, k) in calls:
            r = getattr(h, m)(*a, **k)
        return r
    return f


def build_program(stage="full"):
    nc = bass.Bass("TRN2", target_bir_lowering=False)
    full = stage == "full"
    STOP = int(stage[1:2]) if stage.startswith("s") else 99
    SUB = stage[2:] if stage.startswith("s") else "z"

    def din(name, shape, dt=F32):
        return nc.dram_tensor(name, list(shape), dt, kind="ExternalInput").ap()

    xs = din("xs", [NTILE, SLAB, D])
    kval_d = din("kval", [NTILE, 128, 6])
    icnt_d = din("icnt", [NTILE, 128, 64])
    ebias_d = din("ebias", [128, 3072])
    ident_d = din("ident", [128, 128])
    tri_d = din("tri", [128, 128])
    ones_d = din("ones", [128, 128])
    ecol_d = din("ecol", [128, NE])
    sinks_d = din("sinks", [128, 8])
    ln1g_d = din("ln1g", [128, D])
    ln1b_d = din("ln1b", [128, D])
    pscale_d = din("pscale", [128, 4])
    brout_d = din("brout", [128, NE])
    wr_d = din("wr", [D, NE])
    w_in = din("w_in", [D, 3328])
    wab = din("wab", [512, D])
    wpg = din("wpg", [512, 128])
    wpb = din("wpb", [512, D])
    wout = din("wout", [D, D])
    if full:
        ln2g_d = din("ln2g", [128, D])
        ln2b_d = din("ln2b", [128, D])
        b1T_d = din("b1T", [128, NE * 16])
        b2_d = din("b2", [NE, D])
        w1 = din("w1", [NE, D, 2 * D])
        w2 = din("w2", [NE, D, D])
        out_d = nc.dram_tensor("out", [TOK, D], F32, kind="ExternalOutput").ap()
    xs_scr = nc.dram_tensor("xs_scr", [NE * CAP + 128, D], F32, kind="Internal").ap()
    ys_scr = nc.dram_tensor("ys_scr", [NE * CAP + 128, D], F32, kind="Internal").ap()
    h1_scr = nc.dram_tensor("h1_scr", [TOK, D], F32, kind="Internal" if full else "ExternalOutput").ap()
    if not full:
        dbg_attnT = nc.dram_tensor("dbg_attnT", [128, 2048], F32, kind="ExternalOutput").ap()
        dbg_zsT = nc.dram_tensor("dbg_zsT", [128, 2048], F32, kind="ExternalOutput").ap()
        dbg_mT = nc.dram_tensor("dbg_mT", [128, 4096], F32, kind="ExternalOutput").ap()
        dbg_qT = nc.dram_tensor("dbg_qT", [128, 2048], F32, kind="ExternalOutput").ap()
        dbg_kT = nc.dram_tensor("dbg_kT", [128, 1536], F32, kind="ExternalOutput").ap()
        dbg_v = nc.dram_tensor("dbg_v", [128, 792], F32, kind="ExternalOutput").ap()
        dbg_yT = nc.dram_tensor("dbg_yT", [128, 2048], F32, kind="ExternalOutput").ap()
        dbg_idx = nc.dram_tensor("dbg_idx", [128, 64], I32, kind="ExternalOutput").ap()
        dbg_gates = nc.dram_tensor("dbg_gates", [128, 64], F32, kind="ExternalOutput").ap()

    WNR = 17688
    WNF = 12992
    from contextlib import ExitStack
    with ExitStack() as es:
        def sb(name, shape, dt=F32):
            return es.enter_context(nc.sbuf_tensor(name, list(shape), dt))

        workr = sb("workr", [128, WNR])
        workf = sb("workf", [128, WNF])
        ring_t = sb("ring", [128, NRING * RSLOT], F32R)
        ident = sb("ident_s", [128, 128])
        tri = sb("tri_s", [128, 128])
        ones = sb("ones_s", [128, 128])
        ecol = sb("ecol_s", [128, NE])
        esink = sb("esink", [128, 8])
        pscale = sb("pscale_s", [128, 4])
        brout = sb("brout_s", [128, NE])
        b1T = sb("b1T_s", [128, NE * 16])
        wr = sb("wr_s", [128, 8 * NE])
        runcnt = sb("runcnt", [128, NE])
        gates_all = sb("gates_all", [128, 16 * 4])
        idx_all = sb("idx_all", [128, 16 * 4], I32)
        kval = sb("kval_s", [128, 6])
        icnt = sb("icnt_s", [128, 64])
        small = sb("small", [128, 256])
        psb = [es.enter_context(nc.psum_tensor(f"ps{i}", [128, 512], F32)) for i in range(8)]
        sems = [es.enter_context(nc.semaphore(f"s{i}")) for i in range(80)]
        S = Sched(sems)
        for k_ in ["qT", "kT", "vaug", "PT0", "PT1"] + [f"yT{g_}" for g_ in range(4)]:
            S.fence[k_] = ("FXr", "X")
        S.fence["mT"] = ("FXr", "Y")
        for k_ in ["expS0", "expS1", "attn_tok", "ptmp0", "ptmp1", "h1T"] + [f"pT{g_}" for g_ in range(4)]:
            S.fence[k_] = ("FXf", "X")
        for k_ in ["sa", "spb", "r1_0", "r1_1", "r1_2", "ntmp"]:
            S.fence[k_] = ("FXf", "Y")
        block = es.enter_context(nc.Block())

        def carve(work, off, shape):
            n = int(np.prod(shape[1:]))
            ap = work[:, off:off + n]
            if len(shape) == 3:
                ap = ap.rearrange("p (a b) -> p a b", a=shape[1])
            elif len(shape) == 4:
                ap = ap.rearrange("p (a b c) -> p a b c", a=shape[1], b=shape[2])
            return ap

        def R(ap):
            return ap.bitcast(F32R)

        def r3(ap, a):
            return ap.rearrange("p (a b) -> p a b", a=a)

        psn = [0]
        reserved = set()

        def bank():
            while True:
                i = psn[0] % 8
                psn[0] += 1
                if i not in reserved:
                    return psb[i], f"ps{i}", i

        NR2 = 7
        XOFF = 8320

        def ring(slot):
            if slot < NRING:
                return ring_t[:, slot * RSLOT:(slot + 1) * RSLOT]
            o_ = XOFF + (slot - NRING) * RSLOT
            return workr[:, o_:o_ + RSLOT].bitcast(F32R)

        pieces = []

        slot_of = {}
        next_on_slot = {}

        def emit_piece_load(n):
            slot = slot_of[n]
            sl = ring(slot)
            first = True
            for (dfn, src) in pieces[n]:
                dst = dfn(sl)
                kw = dict(writes=[f"ring{slot}"]) if first else dict(cwrites=[f"ring{slot}"])
                S.op("gp", I("dma_start", out=dst, in_=src), dma=f"ring{slot}", **kw)
                first = False

        def piece_done(n):
            m = next_on_slot.get(n)
            if m is not None:
                emit_piece_load(m)

        offs = {"r": 0, "f": 0}

        def take(shape, a):
            work, lim = (workr, WNR) if a == "r" else (workf, WNF)
            ap = carve(work, offs[a], shape)
            offs[a] += int(np.prod(shape[1:]))
            assert offs[a] <= lim, (a, offs[a])
            return ap

        def tR(shape):
            return take(shape, "r")

        def tF(shape):
            return take(shape, "f")
        xtok = tF([128, 2, D])
        ebias = tF([128, 3, 8, 128])
        ln1g = tF([128, D])
        ln1b = tF([128, D])
        xT = tR([128, 8, SLAB])
        attnT = tR([128, 4, TT])
        zsT = tR([128, 4, TT])
        o_r, o_f = offs["r"], offs["f"]
        qT = tR([128, 4, TT])
        kT0 = tR([128, SLAB])
        kT1 = tR([128, SLAB])
        vaug = tR([128, 6, 2, 66])
        PT2 = tR([128, 2, 512])
        yT = tR([128, 4, TT])
        expS2 = tF([128, 2, 512])
        attn_tok = tF([128, 512])
        pT = tF([128, 4, PWN])
        ptmp = tF([128, 2, PWN])
        h1T = tF([128, 8, 128])
        offs["r"], offs["f"] = o_r, o_f
        mT = tR([128, 8, TT])
        sa = tF([128, TT])
        spb = tF([128, TT])
        r1 = tF([128, 3, D])
        ntmp1 = tF([128, D])

        lg = small[:, 0:32]
        m8 = small[:, 32:40]
        mask = small[:, 40:72]
        posb = small[:, 72:104]
        dest = small[:, 104:136]
        ovf = small[:, 136:168]
        junk = small[:, 168:200]
        destf = small[:, 200:204]
        negm = small[:, 204:205]
        ex4 = small[:, 208:212]
        gs = small[:, 212:213]
        rs = small[:, 213:214]
        stats = small[:, 216:228]
        mv = small[:, 228:230]
        rstd = small[:, 230:231]
        nmr = small[:, 231:232]
        den = small[:, 232:240]
        rec = small[:, 240:248]
        eps_t = small[:, 248:249]

        w_in_v = w_in.rearrange("(kc p) n -> p kc n", p=128)
        wout_v = wout.rearrange("(kc p) n -> p kc n", p=128)
        wab_v = wab.rearrange("(kc p) n -> p kc n", p=128)
        wpb_v = wpb.rearrange("(kc p) n -> p kc n", p=128)
        wpg_v = wpg.rearrange("(g c) d -> c g d", c=128)
        wq_v = w_in[:, 0:512].rearrange("(kc p) (t i d) -> p kc i t d", p=128, t=2, i=4)

        def v3(a, b):
            return lambda sl, a=a, b=b: sl[:, 0:a * b].rearrange("p (a b) -> p a b", a=a)

        def v3o(o_, a, b):
            return lambda sl, o_=o_, a=a, b=b: sl[:, o_:o_ + a * b].rearrange("p (a b) -> p a b", a=a)

        def vq(i, t_):
            return lambda sl, i=i, t_=t_: sl[:, 0:4096].rearrange("p (kc i t d) -> p kc i t d", kc=8, i=4, t=2)[:, :, i, t_, :]

        PI = {}
        for t in range(NTILE):
            PI[(t, "kv")] = len(pieces); pieces.append([(v3(8, 256), w_in_v[:, :, 512:768])])
            PI[(t, "q")] = len(pieces)
            pieces.append([(vq(i, t_), wq_v[:, :, i, t_, :]) for i in range(4) for t_ in range(2)])
            PI[(t, "p")] = len(pieces); pieces.append([(v3(8, 512), w_in_v[:, :, 768:1280])])
            PI[(t, "pg")] = len(pieces); pieces.append([(v3(4, 128), wpg_v)])
            for h_ in range(2):
                PI[(t, "br", h_)] = len(pieces)
                pieces.append([(v3o(0, 4, 512), wab_v[:, :, h_ * 512:(h_ + 1) * 512]),
                               (v3o(2048, 4, 512), wpb_v[:, :, h_ * 512:(h_ + 1) * 512])])
                for jj in (2 * h_, 2 * h_ + 1):
                    PI[(t, "g", jj)] = len(pieces)
                    pieces.append([(v3o(0, 8, 256), w_in_v[:, :, 1280 + jj * 256:1280 + (jj + 1) * 256]),
                                   (v3o(2048, 8, 256), w_in_v[:, :, 2304 + jj * 256:2304 + (jj + 1) * 256])])
            for nh in range(2):
                PI[(t, "wo", nh)] = len(pieces)
                pieces.append([(v3(8, 512), wout_v[:, :, nh * 512:(nh + 1) * 512])])
        if full:
            for e in range(NE):
                w1e = w1[e].rearrange("(kc p) (t g c) -> p kc t g c", p=128, t=2, g=2)
                w2e = w2[e].rearrange("(fc p) n -> p fc n", p=128)
                for cg_ in range(2):
                    for kh in range(2):
                        PI[(e, "w1", cg_, kh)] = len(pieces)
                        pieces.append([(lambda sl, t_=t_: sl[:, 0:4096].rearrange("p (kc t c) -> p kc t c", kc=4, t=2)[:, :, t_, :],
                                        w1e[:, kh * 4:(kh + 1) * 4, t_, cg_, :]) for t_ in range(2)])
                for kk in range(2):
                    PI[(e, "w2", kk)] = len(pieces)
                    pieces.append([(v3(4, 1024), w2e[:, kk * 4:(kk + 1) * 4, :])])

        n0_moe = PI[(0, "w1", 0, 0)] if full else len(pieces)
        last = {}
        for n in range(len(pieces)):
            sl_ = n % NRING if n < n0_moe else (n - n0_moe) % NR2
            slot_of[n] = sl_
            if sl_ in last:
                next_on_slot[last[sl_]] = n
            last[sl_] = n

        def pslot(key):
            n = PI[key]
            return n, ring(slot_of[n]), f"ring{slot_of[n]}"

        def ld(dst, src, key, sem=None):
            S.op("sp", I("dma_start", out=dst, in_=src), writes=[key], dma=(sem or "c_" + key))

        ld(ident[:], ident_d, "ident")
        ld(tri[:], tri_d, "tri")
        ld(ones[:], ones_d, "ones")
        ld(ecol[:], ecol_d, "ecol")
        ld(esink[:], sinks_d, "esink")
        ld(pscale[:], pscale_d, "pscale")
        ld(brout[:], brout_d, "brout")
        if full:
            ld(b1T[:], b1T_d, "b1T")
        ld(r3(wr[:], 8), wr_d.rearrange("(kc p) e -> p kc e", p=128), "wr")
        ld(ebias.rearrange("p a b c -> p (a b c)"), ebias_d, "ebias")
        ld(ln1g, ln1g_d, "ln1g")
        ld(ln1b, ln1b_d, "ln1b")
        for n in range(min(NRING, len(pieces))):
            emit_piece_load(n)
        S.op("act", I("activation", out=esink[:], in_=esink[:], func=AF.Exp), reads=["esink"], writes=["esink"])
        S.op("dve", I("memset", runcnt[:], 0.0), writes=["runcnt"])
        S.op("dve", I("memset", eps_t, LN_EPS), writes=["eps"])
        zf = ptmp.rearrange("p a b -> p (a b)")

        def zero_fill():
            S.op("dve", I("memset", zf, 0.0), writes=["ptmp0", "ptmp1"])
            S.op("dve", I("tensor_copy", R(kT0[64:128, :]), zf[64:128, 0:SLAB]), reads=["ptmp0", "ptmp1"], cwrites=["kT"])
            S.op("dve", I("tensor_copy", R(kT1[0:64, :]), zf[0:64, 0:SLAB]), reads=["ptmp0", "ptmp1"], cwrites=["kT"])
            for hd in range(2):
                S.op("dve", I("tensor_copy", R(vaug[:, :, hd, 65]), zf[:, 0:6]), reads=["ptmp0", "ptmp1"], cwrites=["vaug"])

        alt = [0]

        def evac(out_ap, in_ap, reads, writes=(), cwrites=(), eng=None):
            if eng is None:
                eng = "act" if alt[0] % 2 == 0 else "dve"
                alt[0] += 1
            if eng == "act":
                S.op("act", I("copy", out=out_ap, in_=in_ap), reads=reads, writes=writes, cwrites=cwrites)
            else:
                S.op("dve", I("tensor_copy", out_ap, in_ap), reads=reads, writes=writes, cwrites=cwrites)

        def layer_norm_tok(src, gam, bet, dst, ntmp, key_src, key_dst, gkey, bkey, nkey="ntmp"):
            for c in range(2):
                S.op("dve", I("bn_stats", out=stats[:, c * 6:(c + 1) * 6], in_=src[:, c * 512:(c + 1) * 512]),
                     reads=[key_src], cwrites=["stats"])
            S.op("dve", I("bn_aggr", out=mv, in_=stats), reads=["stats"], writes=["mv"])
            S.op("act", I("activation", out=rstd, in_=mv[:, 1:2], func=AF.Ln, bias=eps_t, scale=1.0),
                 reads=["mv", "eps"], writes=["rstd"])
            S.op("act", I("activation", out=rstd, in_=rstd, func=AF.Exp, scale=-0.5), reads=["rstd"], writes=["rstd"])
            S.op("dve", I("tensor_scalar", out=nmr, in0=mv[:, 0:1], scalar1=rstd, scalar2=-1.0,
                          op0=ALU.mult, op1=ALU.mult), reads=["mv", "rstd"], writes=["nmr", "stats"])
            S.op("act", I("activation", out=ntmp, in_=src, func=AF.Identity, bias=nmr, scale=rstd),
                 reads=[key_src, "rstd", "nmr"], writes=[nkey])
            S.op("dve", I("tensor_tensor", out=ntmp, in0=ntmp, in1=gam, op=ALU.mult),
                 reads=[nkey, gkey], writes=[nkey])
            S.op("dve", I("tensor_tensor", out=dst, in0=ntmp, in1=bet, op=ALU.add),
                 reads=[nkey, bkey], writes=[key_dst])

        def transp4(dst_bank, src_aps):
            return G([("transpose", (), dict(out=dst_bank[:, c * 128:(c + 1) * 128], in_=a, identity=ident[:]))
                      for c, a in enumerate(src_aps)])

        def mmgroup(out_ap, pairs):
            n = len(pairs)
            return G([("matmul", (out_ap, l, r), dict(start=(i == 0), stop=(i == n - 1)))
                      for i, (l, r) in enumerate(pairs)])

        wrv = r3(wr[:], 8)

        def router(t):
            for tb in range(4):
                tbg = t * 4 + tb
                rb = xtok[:, tb % 2, :]
                rk = f"xtok{tb % 2}"
                S.op("sp", I("dma_start", out=rb, in_=h1_scr[tbg * 128:(tbg + 1) * 128, :]),
                     reads=["h1_scr"], writes=[rk], dma=rk)
                for hb in range(2):
                    pb_, pk, _ = bank()
                    S.op("pe", transp4(pb_, [rb[:, (hb * 4 + c) * 128:(hb * 4 + c + 1) * 128] for c in range(4)]),
                         reads=[rk, "ident"], writes=[pk])
                    evac(h1T[:, hb * 4:(hb + 1) * 4, :], r3(pb_[:, :], 4), reads=[pk], cwrites=["h1T"], eng="act")
                pb_, pk, _ = bank()
                S.op("pe", mmgroup(pb_[:, 0:32], [(h1T[:, kc, :], wrv[:, kc, :]) for kc in range(8)]),
                     reads=["h1T", "wr"], writes=[pk])
                S.op("dve", I("tensor_tensor", out=lg, in0=pb_[:, 0:32], in1=brout[:], op=ALU.add),
                     reads=[pk, "brout"], writes=["lg"])
                S.op("dve", I("max", out=m8, in_=lg), reads=["lg"], writes=["m8"])
                S.op("dve", I("tensor_scalar", out=mask, in0=lg, scalar1=m8[:, 3:4], scalar2=None, op0=ALU.is_ge),
                     reads=["lg", "m8"], writes=["mask"])
                pb2, pk2, _ = bank()
                S.op("pe", I("matmul", pb2[:, 0:32], tri[:], mask, start=True, stop=True), reads=["tri", "mask"], writes=[pk2])
                S.op("pe", I("matmul", pb2[:, 32:64], ones[:], mask, start=True, stop=True), reads=["ones", "mask"], cwrites=[pk2])
                S.op("dve", I("tensor_tensor", out=posb, in0=pb2[:, 0:32], in1=runcnt[:], op=ALU.add),
                     reads=[pk2, "runcnt"], writes=["posb"])
                S.op("dve", I("tensor_tensor", out=runcnt[:], in0=runcnt[:], in1=pb2[:, 32:64], op=ALU.add),
                     reads=[pk2, "runcnt"], writes=["runcnt"])
                S.op("dve", I("tensor_scalar", out=ovf, in0=posb, scalar1=float(CAP), scalar2=None, op0=ALU.is_ge),
                     reads=["posb"], writes=["ovf"])
                S.op("dve", I("tensor_tensor", out=dest, in0=posb, in1=ecol[:], op=ALU.add),
                     reads=["posb", "ecol"], writes=["dest"])
                S.op("dve", I("tensor_scalar", out=posb, in0=dest, scalar1=-1.0, scalar2=float(NE * CAP),
                              op0=ALU.mult, op1=ALU.add), reads=["dest"], writes=["posb"])
                S.op("dve", I("tensor_tensor", out=posb, in0=posb, in1=ovf, op=ALU.mult), reads=["posb", "ovf"], writes=["posb"])
                S.op("dve", I("tensor_tensor", out=dest, in0=dest, in1=posb, op=ALU.add), reads=["dest", "posb"], writes=["dest"])
                for k in range(4):
                    S.op("dve", I("scalar_tensor_tensor", out=junk, in0=lg, scalar=m8[:, k:k + 1], in1=dest,
                                  op0=ALU.is_equal, op1=ALU.mult, accum_out=destf[:, k:k + 1]),
                         reads=["lg", "m8", "dest"], writes=["junk"], cwrites=["destf"])
                S.op("dve", I("tensor_copy", idx_all[:, tbg * 4:(tbg + 1) * 4], destf), reads=["destf"], writes=[f"idx{tbg}"])
                S.op("dve", I("tensor_scalar", out=negm, in0=m8[:, 0:1], scalar1=-1.0, scalar2=None, op0=ALU.mult),
                     reads=["m8"], writes=["negm"])
                S.op("act", I("activation", out=ex4, in_=m8[:, 0:4], func=AF.Exp, bias=negm, scale=1.0, accum_out=gs),
                     reads=["m8", "negm"], writes=["ex4", "gs"])
                S.op("dve", I("reciprocal", out=rs, in_=gs), reads=["gs"], writes=["rs"])
                S.op("dve", I("tensor_scalar", out=gates_all[:, tbg * 4:(tbg + 1) * 4], in0=ex4, scalar1=rs,
                              scalar2=None, op0=ALU.mult), reads=["ex4", "rs"], writes=[f"gates{tbg}"])
                for k in range(4 if stage != "h1ns" else 0):
                    S.op("gp", I("indirect_dma_start", out=xs_scr,
                                 out_offset=bass.IndirectOffsetOnAxis(ap=idx_all[:, tbg * 4 + k:tbg * 4 + k + 1], axis=0),
                                 in_=rb, in_offset=None),
                         reads=[rk, f"idx{tbg}"], cwrites=["xs_scr"], dma=f"scat{tb % 2}")


        def T1_blocks(t, blks):
            for blk in blks:
                xb = xtok[:, blk % 2, :]
                xk = f"xtok{blk % 2}"
                S.op("sp", I("dma_start", out=xb, in_=xs[t, blk * 128:(blk + 1) * 128, :]), writes=[xk], dma=xk)
                for hb in range(2):
                    pb_, pk, _ = bank()
                    S.op("pe", transp4(pb_, [xb[:, (hb * 4 + c) * 128:(hb * 4 + c + 1) * 128] for c in range(4)]),
                         reads=[xk, "ident"], writes=[pk])
                    evac(R(xT[:, hb * 4:(hb + 1) * 4, blk * 128:(blk + 1) * 128]), r3(pb_[:, :], 4),
                         reads=[pk], cwrites=[f"xT{blk}"], eng=("act" if t > 0 else None))

        for t in range(NTILE):
            ld(kval[:], kval_d[t], "kval", sem="kval")
            ld(icnt[:], icnt_d[t], "icnt", sem="icnt")
            zero_fill()
            if t == 0:
                T1_blocks(0, range(6))
            xTall = [f"xT{b}" for b in range(6)]
            if STOP <= 1:
                break
            n_kv, s_kv, k_kv = pslot((t, "kv"))
            kvv = r3(s_kv[:, 0:2048], 8)
            for hh in range(2):
                pb_, pk, _ = bank()
                S.op("pe", mmgroup(pb_[:, 0:384], [(kvv[:, kc, 0:128], R(xT[:, kc, hh * 384:(hh + 1) * 384]))
                                                   for kc in range(8)]), reads=[k_kv] + xTall, writes=[pk])
                if SUB >= "b":
                    ee = "act" if (hh == 0 or t > 0) else "dve"
                    evac(R(kT0[0:64, hh * 384:(hh + 1) * 384]), pb_[0:64, 0:384], reads=[pk], cwrites=["kT"], eng=ee)
                    evac(R(kT1[64:128, hh * 384:(hh + 1) * 384]), pb_[64:128, 0:384], reads=[pk], cwrites=["kT"], eng=ee)
            for half in range(2 if SUB >= "c" else 0):
                pb_, pk, _ = bank()
                calls = []
                for b3 in range(3):
                    blk = half * 3 + b3
                    for kc in range(8):
                        calls.append(("matmul", (pb_[:, b3 * 128:(b3 + 1) * 128], R(xT[:, kc, blk * 128:(blk + 1) * 128]),
                                                 kvv[:, kc, 128:256]), dict(start=(kc == 0), stop=(kc == 7))))
                S.op("pe", G(calls), reads=[k_kv] + xTall, writes=[pk])
                for b3 in range(3 if SUB >= "d" else 0):
                    blk = half * 3 + b3
                    evac(R(vaug[:, blk, :, 0:64]), r3(pb_[:, b3 * 128:(b3 + 1) * 128], 2), reads=[pk], cwrites=["vaug"],
                         eng=("act" if (half == 0 or t > 0) else "dve"))
            piece_done(n_kv)
            for hd in range(2 if SUB >= "e" else 0):
                S.op("dve", I("tensor_copy", R(vaug[:, :, hd, 64]), kval[:, :]), reads=["kval"], cwrites=["vaug"])
            if STOP <= 2 or (STOP == 3 and SUB != ''):
                break
            n_q, s_q, k_q = pslot((t, "q"))
            qv = r3(s_q[:, 0:4096], 8)
            for i4 in range(4):
                pb_, pk, _ = bank()
                S.op("pe", mmgroup(pb_[:, :], [(qv[:, kc, i4 * 128:(i4 + 1) * 128], R(xT[:, kc, 128:640]))
                                               for kc in range(8)]), reads=[k_q] + xTall, writes=[pk])
                evac(R(qT[:, i4, :]), pb_[:, :], reads=[pk], cwrites=["qT"], eng=("act" if t > 0 else None))
            piece_done(n_q)
            if STOP <= 3:
                break
            n_p, s_p, k_p = pslot((t, "p"))
            pv_ = r3(s_p[:, 0:4096], 8)
            for g in range(4):
                for hh in range(2):
                    pb_, pk, _ = bank()
                    S.op("pe", mmgroup(pb_[:, 0:272], [(pv_[:, kc, g * 128:(g + 1) * 128],
                                                        R(xT[:, kc, PW0 + hh * 272:PW0 + (hh + 1) * 272]))
                                                       for kc in range(8)]), reads=[k_p] + xTall, writes=[pk])
                    evac(pT[:, g, hh * 272:(hh + 1) * 272], pb_[:, 0:272], reads=[pk], cwrites=[f"pT{g}"],
                         eng=("act" if t > 0 else None))
            piece_done(n_p)
            if t > 0:
                router(t - 1)
            if not full and t == 0:
                S.op("sp", I("dma_start", out=dbg_qT, in_=qT.rearrange("p a b -> p (a b)")), reads=["qT"], dma="dbg")
                S.op("sp", I("dma_start", out=dbg_kT[:, 0:768], in_=kT0), reads=["kT"], dma="dbg")
                S.op("sp", I("dma_start", out=dbg_kT[:, 768:1536], in_=kT1), reads=["kT"], dma="dbg")
                S.op("sp", I("dma_start", out=dbg_v, in_=vaug.rearrange("p a b c -> p (a b c)")), reads=["vaug"], dma="dbg")
            if STOP <= 5:
                break
            pool_ops = []

            def PQ(eng, fn, **kw):
                pool_ops.append((eng, fn, kw))

            def pool_drip(n):
                for _ in range(n):
                    if pool_ops:
                        e_, f_, kw_ = pool_ops.pop(0)
                        S.op(e_, f_, **kw_)

            for g in range(4):
                cur = pT[:, g, :]
                curk = f"pT{g}"
                lo, hi = 8, PWN - 8
                for l in range(1, g + 2):
                    dst = ptmp[:, l % 2, :]
                    dk = f"ptmp{l % 2}"
                    if l == 1:
                        PQ("dve", I("tensor_tensor", out=dst[:, lo:hi], in0=cur[:, lo - 1:hi - 1], in1=cur[:, lo:hi],
                                      op=ALU.add), reads=[curk], writes=[dk])
                    else:
                        sh = 2 ** (l - 2)
                        PQ("dve", I("tensor_tensor", out=dst[:, lo:hi], in0=cur[:, lo - sh:hi - sh],
                                      in1=cur[:, lo + sh:hi + sh], op=ALU.add), reads=[curk], writes=[dk])
                    cur, curk = dst, dk
                w = 2 ** (g + 1)
                PQ("dve", I("scalar_tensor_tensor", out=R(yT[:, g, :]), in0=cur[:, 16:528], scalar=1.0 / w,
                              in1=pT[:, g, 16:528], op0=ALU.mult, op1=ALU.subtract),
                     reads=[curk, f"pT{g}"], writes=[f"yT{g}"])
                for ed in range(2):
                    u0 = 16 if ed == 0 else 520
                    y0 = 0 if ed == 0 else 504
                    ic = icnt[:, g * 16 + ed * 8:g * 16 + ed * 8 + 8]
                    PQ("dve", I("tensor_tensor", out=junk[:, 0:8], in0=cur[:, u0:u0 + 8], in1=ic, op=ALU.mult),
                         reads=[curk, "icnt"], writes=["junk"])
                    PQ("dve", I("tensor_tensor", out=R(yT[:, g, y0:y0 + 8]), in0=junk[:, 0:8], in1=pT[:, g, u0:u0 + 8],
                                  op=ALU.subtract), reads=["junk", f"pT{g}"], cwrites=[f"yT{g}"])
            if STOP <= 4:
                break
            pvbs = {}

            def att_S(qb, g, jj):
                if qb not in pvbs:
                    pvbs[qb] = [bank(), bank()]
                    reserved.update([pvbs[qb][0][2], pvbs[qb][1][2]])
                km = kT0 if g == 0 else kT1
                kb = qb + jj
                pb_, pk, _ = bank()
                S.op("pe", I("matmul", pb_[:, :], R(km[:, kb * 128:(kb + 1) * 128]), R(qT[:, :, qb * 128:(qb + 1) * 128]),
                             start=True, stop=True), reads=["kT", "qT"], writes=[pk])
                pp = (qb * 6 + g * 3 + jj) % 2
                S.op("act", I("activation", out=expS2[:, pp, :], in_=pb_[:, :], func=AF.Exp, scale=0.125),
                     reads=[pk], writes=[f"expS{pp}"])
                S.op("dve", I("tensor_tensor", out=r3(R(PT2[:, pp, :]), 4), in0=r3(expS2[:, pp, :], 4),
                              in1=ebias[:, jj, 4 * g:4 * g + 4, :], op=ALU.mult), reads=[f"expS{pp}", "ebias"], writes=[f"PT{pp}"])

            def att_PV(qb, g, jj):
                pp = (qb * 6 + g * 3 + jj) % 2
                PT = PT2[:, pp, :]
                pvt, pvk, _ = pvbs[qb][g]
                calls = [("matmul", (pvt[:, i * 66:(i + 1) * 66], R(PT[:, i * 128:(i + 1) * 128]), R(vaug[:, qb + jj, g, :])),
                          dict(start=(jj == 0 and i == 0), stop=(jj == 2 and i == 3))) for i in range(4)]
                if jj == 0:
                    S.op("pe", G(calls), reads=[f"PT{pp}", "vaug"], writes=[pvk])
                else:
                    S.op("pe", G(calls), reads=[f"PT{pp}", "vaug"], cwrites=[pvk])

            def att_N(qb):
                pvb = pvbs[qb]
                for hb in range(2):
                    pvt, pvk, _ = pvb[hb]
                    S.op("dve", I("tensor_tensor", out=den[:, hb * 4:(hb + 1) * 4], in0=r3(pvt[:, 0:264], 4)[:, :, 64],
                                  in1=esink[:, hb * 4:(hb + 1) * 4], op=ALU.add), reads=[pvk, "esink"], cwrites=["den"])
                S.op("dve", I("reciprocal", out=rec, in_=den), reads=["den"], writes=["rec"])
                for hd in range(8):
                    pvt, pvk, _ = pvb[hd // 4]
                    c0 = (hd % 4) * 66
                    if hd // 4 == 0:
                        S.op("act", I("activation", out=attn_tok[:, hd * 64:(hd + 1) * 64], in_=pvt[:, c0:c0 + 64],
                                      func=AF.Identity, scale=rec[:, hd:hd + 1]), reads=[pvk, "rec"], cwrites=["attn_tok"])
                    else:
                        S.op("dve", I("tensor_scalar", out=attn_tok[:, hd * 64:(hd + 1) * 64], in0=pvt[:, c0:c0 + 64],
                                      scalar1=rec[:, hd:hd + 1], scalar2=None, op0=ALU.mult),
                             reads=[pvk, "rec"], cwrites=["attn_tok"])
                reserved.discard(pvb[0][2])
                reserved.discard(pvb[1][2])

            def att_T(qb):
                pb_, pk, _ = bank()
                S.op("pe", transp4(pb_, [attn_tok[:, c * 128:(c + 1) * 128] for c in range(4)]),
                     reads=["attn_tok", "ident"], writes=[pk])
                evac(R(attnT[:, :, qb * 128:(qb + 1) * 128]), r3(pb_[:, :], 4), reads=[pk], cwrites=["attnT"], eng="act")

            steps = [(qb, g, jj) for qb in range(4) for g in range(2) for jj in range(3)]
            att_S(*steps[0])
            for si, (qb, g, jj) in enumerate(steps):
                if si + 1 < len(steps):
                    att_S(*steps[si + 1])
                att_PV(qb, g, jj)
                pool_drip(2)
                if g == 0 and jj == 1 and qb > 0:
                    att_T(qb - 1)
                if g == 1 and jj == 2:
                    att_N(qb)
            att_T(3)
            pool_drip(1000)
            if not full and t == 0:
                S.op("sp", I("dma_start", out=dbg_yT, in_=yT.rearrange("p a b -> p (a b)")), reads=[f"yT{g_}" for g_ in range(4)], dma="dbg")
            if STOP <= 6:
                break
            n_pg, s_pg, k_pg = pslot((t, "pg"))
            pgv = r3(s_pg[:, 0:512], 4)
            for g in range(4):
                pb_, pk, _ = bank()
                S.op("pe", I("matmul", pb_[:, :], pgv[:, g, :], R(yT[:, g, :]), start=True, stop=True),
                     reads=[k_pg, f"yT{g}"], writes=[pk])
                S.op("act", I("activation", out=R(zsT[:, g, :]), in_=pb_[:, :], func=AF.Identity, scale=pscale[:, g:g + 1]),
                     reads=[pk, "pscale"], cwrites=["zsT"])
            piece_done(n_pg)
            if STOP <= 7:
                break
            for j in range(8):
                n_g, s_g, k_g = pslot((t, "g", j // 2))
                n_b, s_b, k_b = pslot((t, "br", j // 4))
                cb = (j % 4) * 128
                cg = (j % 2) * 128
                abv = r3(s_b[:, 0:2048], 4)[:, :, cb:cb + 128]
                pbv = r3(s_b[:, 2048:4096], 4)[:, :, cb:cb + 128]
                gav = r3(s_g[:, 0:2048], 8)[:, :, cg:cg + 128]
                gpv = r3(s_g[:, 2048:4096], 8)[:, :, cg:cg + 128]
                bA, kA, _ = bank(); bB, kB, _ = bank(); bC, kC, _ = bank(); bD, kD, _ = bank()
                S.op("pe", mmgroup(bC[:, :], [(gav[:, kc, :], R(xT[:, kc, 128:640])) for kc in range(8)]),
                     reads=[k_g] + xTall, writes=[kC])
                S.op("pe", mmgroup(bD[:, :], [(gpv[:, kc, :], R(xT[:, kc, 128:640])) for kc in range(8)]),
                     reads=[k_g] + xTall, writes=[kD])
                S.op("pe", mmgroup(bA[:, :], [(abv[:, c, :], R(attnT[:, c, :])) for c in range(4)]),
                     reads=[k_b, "attnT"], writes=[kA])
                S.op("pe", mmgroup(bB[:, :], [(pbv[:, c, :], R(zsT[:, c, :])) for c in range(4)]),
                     reads=[k_b, "zsT"], writes=[kB])
                if j % 2 == 1:
                    piece_done(n_g)
                if j % 4 == 3:
                    piece_done(n_b)
                S.op("act", I("activation", out=sa, in_=bC[:, :], func=AF.Sigmoid), reads=[kC], writes=["sa"])
                S.op("act", I("activation", out=spb, in_=bD[:, :], func=AF.Sigmoid), reads=[kD], writes=["spb"])
                S.op("dve", I("tensor_tensor", out=sa, in0=sa, in1=bA[:, :], op=ALU.mult), reads=["sa", kA], writes=["sa"])
                S.op("dve", I("tensor_tensor", out=spb, in0=spb, in1=bB[:, :], op=ALU.mult), reads=["spb", kB], writes=["spb"])
                S.op("dve", I("tensor_tensor", out=R(mT[:, j, :]), in0=sa, in1=spb, op=ALU.add),
                     reads=["sa", "spb"], cwrites=["mT"])
            if not full and t == 0:
                S.op("sp", I("dma_start", out=dbg_attnT, in_=attnT.rearrange("p a b -> p (a b)")), reads=["attnT"], dma="dbg")
                S.op("sp", I("dma_start", out=dbg_zsT, in_=zsT.rearrange("p a b -> p (a b)")), reads=["zsT"], dma="dbg")
                S.op("sp", I("dma_start", out=dbg_mT, in_=mT.rearrange("p a b -> p (a b)")), reads=["mT"], dma="dbg")
            if STOP <= 8:
                break
            n_w0, s_w0, k_w0 = pslot((t, "wo", 0))
            n_w1, s_w1, k_w1 = pslot((t, "wo", 1))
            wov = [r3(s_w0[:, 0:4096], 8), r3(s_w1[:, 0:4096], 8)]
            wok = [k_w0, k_w1]
            def stageA(tb):
                rb = r1[:, tb % 3, :]
                rk = f"r1_{tb % 3}"
                S.op("gp", I("dma_start", out=rb, in_=xs[t, 128 + tb * 128:128 + (tb + 1) * 128, :]),
                     writes=[rk], dma=rk + "ld")
                for nh in range(2):
                    pb_, pk, _ = bank()
                    S.op("pe", mmgroup(pb_[:, :], [(R(mT[:, j, tb * 128:(tb + 1) * 128]), wov[nh][:, j, :]) for j in range(8)]),
                         reads=[wok[nh], "mT"], writes=[pk])
                    S.op("dve", I("scalar_tensor_tensor", out=rb[:, nh * 512:(nh + 1) * 512], in0=rb[:, nh * 512:(nh + 1) * 512],
                                  scalar=ALPHA, in1=pb_[:, :], op0=ALU.mult, op1=ALU.add), reads=[pk, rk], cwrites=[rk])
                if tb == 3:
                    piece_done(n_w0)
                    piece_done(n_w1)

            stageA(0)
            for tb in range(4):
                tbg = t * 4 + tb
                rb = r1[:, tb % 3, :]
                rk = f"r1_{tb % 3}"
                if tb + 1 < 4:
                    stageA(tb + 1)
                layer_norm_tok(rb, ln1g, ln1b, rb, ntmp1, rk, rk, "ln1g", "ln1b")
                S.op("gp", I("dma_start", out=h1_scr[tbg * 128:(tbg + 1) * 128, :], in_=rb),
                     reads=[rk], cwrites=["h1_scr"], dma=rk + "st")
                if t + 1 < NTILE and STOP == 99:
                    T1_blocks(t + 1, {0: [0, 1], 1: [2, 3], 2: [4], 3: [5]}[tb])
            if t == NTILE - 1:
                router(t)

        if not full and STOP == 99:
            S.op("sp", I("dma_start", out=dbg_idx, in_=idx_all[:]), reads=[f"idx{i}" for i in range(16)], dma="dbg")
            S.op("sp", I("dma_start", out=dbg_gates, in_=gates_all[:]), reads=[f"gates{i}" for i in range(16)], dma="dbg")

        if full:
            S.barrier()
            offs["r"], offs["f"] = 0, 0
            xs_tok = tF([128, 3, D])
            xsT = tR([128, 8, CAP])
            actT = tR([128, 8, CAP])
            gbuf = tF([128, 2, CAP])
            sgb = tF([128, 2, CAP])
            ubuf = tF([128, 2, CAP])
            y_tok = tF([128, 3, D])
            b2bc = tF([128, 2, D])
            E0 = tR([128, 128])
            b2pad = tR([128, 2, D])
            assert offs["r"] <= XOFF

            def load_xs(e):
                S.op("sp", I("dma_start", out=xs_tok,
                             in_=xs_scr[e * CAP:(e + 1) * CAP, :].rearrange("(b p) d -> p b d", p=128)),
                     reads=["xs_scr"], writes=["xs_tok"], dma="xs_tok")

            def transposes(e):
                for kc in range(8):
                    pb_, pk, _ = bank()
                    S.op("pe", G([("transpose", (), dict(out=pb_[:, b * 128:(b + 1) * 128],
                                                         in_=xs_tok[:, b, kc * 128:(kc + 1) * 128], identity=ident[:]))
                                  for b in range(3)]), reads=["xs_tok", "ident"], writes=[pk])
                    evac(R(xsT[:, kc, :]), pb_[:, 0:CAP], reads=[pk], cwrites=["xsT"], eng="act")

            def mlp1(e):
                S.op("gp", I("dma_start", out=R(b2pad[0:1, e % 2, :]), in_=b2_d[e:e + 1, :]),
                     writes=[f"b2pad{e % 2}"], dma=f"b2pad{e % 2}")
                for cg_ in range(2):
                    n_a, s_a, k_a = pslot((e, "w1", cg_, 0))
                    n_b, s_b, k_b = pslot((e, "w1", cg_, 1))
                    wh = [s_a[:, 0:4096].rearrange("p (kc t c) -> p kc t c", kc=4, t=2),
                          s_b[:, 0:4096].rearrange("p (kc t c) -> p kc t c", kc=4, t=2)]
                    for ii in range(4):
                        i8 = cg_ * 4 + ii
                        bG, kG, _ = bank(); bU, kU, _ = bank()
                        S.op("pe", mmgroup(bG[:, 0:CAP], [(wh[kc // 4][:, kc % 4, 0, ii * 128:(ii + 1) * 128], R(xsT[:, kc, :]))
                                                          for kc in range(8)]), reads=[k_a, k_b, "xsT"], writes=[kG])
                        S.op("pe", mmgroup(bU[:, 0:CAP], [(wh[kc // 4][:, kc % 4, 1, ii * 128:(ii + 1) * 128], R(xsT[:, kc, :]))
                                                          for kc in range(8)]), reads=[k_a, k_b, "xsT"], writes=[kU])
                        p2 = i8 % 2
                        gb = gbuf[:, p2, :]; sg = sgb[:, p2, :]; ub = ubuf[:, p2, :]
                        cg = e * 16 + i8
                        cu = e * 16 + 8 + i8
                        S.op("dve", I("tensor_scalar", out=gb, in0=bG[:, 0:CAP], scalar1=b1T[:, cg:cg + 1], scalar2=7.0,
                                      op0=ALU.add, op1=ALU.min), reads=[kG, "b1T"], writes=[f"gb{p2}"])
                        S.op("act", I("activation", out=sg, in_=gb, func=AF.Sigmoid, scale=1.702),
                             reads=[f"gb{p2}"], writes=[f"sg{p2}"])
                        S.op("dve", I("tensor_scalar", out=ub, in0=bU[:, 0:CAP], scalar1=b1T[:, cu:cu + 1], scalar2=7.0,
                                      op0=ALU.add, op1=ALU.min), reads=[kU, "b1T"], writes=[f"ub{p2}"])
                        S.op("dve", I("tensor_scalar", out=ub, in0=ub, scalar1=-7.0, scalar2=1.0, op0=ALU.max, op1=ALU.add),
                             reads=[f"ub{p2}"], writes=[f"ub{p2}"])
                        S.op("dve", I("tensor_tensor", out=gb, in0=gb, in1=sg, op=ALU.mult),
                             reads=[f"gb{p2}", f"sg{p2}"], writes=[f"gb{p2}"])
                        S.op("dve", I("tensor_tensor", out=R(actT[:, i8, :]), in0=ub, in1=gb, op=ALU.mult),
                             reads=[f"gb{p2}", f"ub{p2}"], cwrites=["actT"])
                    piece_done(n_a)
                    piece_done(n_b)

            def mlp2(e):
                n_a, s_a, k_a = pslot((e, "w2", 0))
                n_b, s_b, k_b = pslot((e, "w2", 1))
                w2v = [r3(s_a[:, 0:4096], 4), r3(s_b[:, 0:4096], 4)]
                bb = b2bc[:, e % 2, :]
                bk = f"b2bc{e % 2}"
                for nh in range(2):
                    pbb, pkb, _ = bank()
                    S.op("pe", I("matmul", pbb[:, :], R(E0[:, :]), R(b2pad[:, e % 2, nh * 512:(nh + 1) * 512]), start=True, stop=True),
                         reads=["E0", f"b2pad{e % 2}"], writes=[pkb])
                    kwb = dict(writes=[bk]) if nh == 0 else dict(cwrites=[bk])
                    S.op("act", I("copy", out=bb[:, nh * 512:(nh + 1) * 512], in_=pbb[:, :]), reads=[pkb], **kwb)
                for b in range(3):
                    yb = y_tok[:, b, :]
                    yk = f"ytok{b}"
                    for nh in range(2):
                        pb_, pk, _ = bank()
                        S.op("pe", mmgroup(pb_[:, :], [(R(actT[:, fc, b * 128:(b + 1) * 128]),
                                                        w2v[fc // 4][:, fc % 4, nh * 512:(nh + 1) * 512]) for fc in range(8)]),
                             reads=[k_a, k_b, "actT"], writes=[pk])
                        kw = dict(writes=[yk]) if nh == 0 else dict(cwrites=[yk])
                        S.op("dve", I("tensor_tensor", out=yb[:, nh * 512:(nh + 1) * 512], in0=pb_[:, :],
                                      in1=bb[:, nh * 512:(nh + 1) * 512], op=ALU.add), reads=[pk, bk], **kw)
                    S.op("sp", I("dma_start", out=ys_scr[e * CAP + b * 128:e * CAP + (b + 1) * 128, :], in_=yb),
                         reads=[yk], cwrites=["ys_scr"], dma=yk)
                piece_done(n_a)
                piece_done(n_b)

            for n in range(n0_moe, len(pieces)):
                if slot_of[n] >= NRING and (n - n0_moe) < NR2:
                    emit_piece_load(n)
            S.op("dve", I("memset", y_tok[:, 0, :], 0.0), writes=["ytok0"])
            S.op("dve", I("memset", y_tok[:, 1, :], 0.0), writes=["ytok1"])
            for i_ in range(2):
                S.op("dve", I("tensor_copy", R(b2pad[:, i_, :]), y_tok[:, i_, :]), reads=[f"ytok{i_}"], writes=[f"b2pad{i_}"])
            S.op("dve", I("tensor_copy", y_tok[0:1, 1, 0:128], ones[0:1, :]), reads=["ones"], writes=["ytok1"])
            S.op("dve", I("tensor_copy", R(E0[:, :]), y_tok[:, 1, 0:128]), reads=["ytok1"], writes=["E0"])
            S.op("sp", I("dma_start", out=ys_scr[NE * CAP:NE * CAP + 128, :], in_=y_tok[:, 0, :]),
                 reads=["ytok0"], cwrites=["ys_scr"], dma="ytok0")
            load_xs(0)
            transposes(0)
            for e in range(NE):
                mlp1(e)
                if e + 1 < NE:
                    load_xs(e + 1)
                    transposes(e + 1)
                mlp2(e)

            S.barrier()
            offs["r"], offs["f"] = 0, 0
            yg = tR([128, 2, 4, D])
            dg = tR([128, 2, 4, 128])
            ln2g = tF([128, D])
            ln2b = tF([128, D])
            hb2 = tF([128, 2, D])
            ob2 = tF([128, 2, D])
            ld(ln2g, ln2g_d, "ln2g")
            ld(ln2b, ln2b_d, "ln2b")

            def gathers(tbg):
                p2 = tbg % 2
                for k in range(4):
                    kw = dict(writes=[f"yg{p2}"]) if k == 0 else dict(cwrites=[f"yg{p2}"])
                    S.op("gp", I("indirect_dma_start", out=R(yg[:, p2, k, :]), out_offset=None, in_=ys_scr,
                                 in_offset=bass.IndirectOffsetOnAxis(ap=idx_all[:, tbg * 4 + k:tbg * 4 + k + 1], axis=0)),
                         reads=["ys_scr", f"idx{tbg}"], dma=f"yg{p2}", **kw)
                S.op("sp", I("dma_start", out=hb2[:, p2, :], in_=h1_scr[tbg * 128:(tbg + 1) * 128, :]),
                     reads=["h1_scr"], writes=[f"hb{p2}"], dma=f"hb{p2}")

            def combine_mm(tbg):
                p2 = tbg % 2
                for k in range(4):
                    kw = dict(writes=[f"dg{p2}"]) if k == 0 else dict(cwrites=[f"dg{p2}"])
                    S.op("dve", I("tensor_scalar", out=R(dg[:, p2, k, :]), in0=ident[:],
                                  scalar1=gates_all[:, tbg * 4 + k:tbg * 4 + k + 1], scalar2=None, op0=ALU.mult),
                         reads=["ident", f"gates{tbg}"], **kw)
                res = []
                for nh in range(2):
                    pb_, pk, bi = bank()
                    reserved.add(bi)
                    S.op("pe", mmgroup(pb_[:, :], [(R(dg[:, p2, k, :]), R(yg[:, p2, k, nh * 512:(nh + 1) * 512])) for k in range(4)]),
                         reads=[f"dg{p2}", f"yg{p2}"], writes=[pk])
                    res.append((pb_, pk, bi))
                return res

            gathers(0)
            gathers(1)
            pend = combine_mm(0)
            for tbg in range(16):
                p2 = tbg % 2
                cur = pend
                if tbg + 1 < 16:
                    pend = combine_mm(tbg + 1)
                hb = hb2[:, p2, :]
                hk = f"hb{p2}"
                for nh in range(2):
                    pb_, pk, bi = cur[nh]
                    S.op("dve", I("scalar_tensor_tensor", out=hb[:, nh * 512:(nh + 1) * 512], in0=hb[:, nh * 512:(nh + 1) * 512],
                                  scalar=ALPHA, in1=pb_[:, :], op0=ALU.mult, op1=ALU.add), reads=[pk, hk], cwrites=[hk])
                    reserved.discard(bi)
                ob = ob2[:, p2, :]
                ok_ = f"ob{p2}"
                layer_norm_tok(hb, ln2g, ln2b, ob, ob, hk, ok_, "ln2g", "ln2b", nkey=ok_)
                if tbg + 2 < 16:
                    gathers(tbg + 2)
                S.op("sp", I("dma_start", out=out_d[tbg * 128:(tbg + 1) * 128, :], in_=ob),
                     reads=[ok_], cwrites=["out"], dma=f"outst{p2}")

        S.final_waits("sp")

        @block.sync
        def _(h):
            S.replay("sp", h)

        @block.tensor
        def _(h):
            S.replay("pe", h)

        @block.scalar
        def _(h):
            S.replay("act", h)

        @block.vector
        def _(h):
            S.replay("dve", h)

        @block.gpsimd
        def _(h):
            S.replay("gp", h)
    return nc


def _consts():
    slopes = np.asarray([2.0 ** (-8.0 * (h + 1) / 8) for h in range(8)], np.float64)
    kk = np.arange(128)[:, None]
    qq = np.arange(128)[None, :]
    eb = np.zeros((128, 3, 8, 128), np.float64)
    for jj in range(3):
        rel = kk + (jj - 1) * 128 - qq
        valid = np.abs(rel) <= 128
        for h in range(8):
            eb[:, jj, h, :] = np.where(valid, np.exp(-slopes[h] * np.abs(rel)), 0.0)
    ident = np.eye(128, dtype=np.float32)
    tri = (np.arange(128)[:, None] < np.arange(128)[None, :]).astype(np.float32)
    ones = np.ones((128, 128), np.float32)
    ecol = np.broadcast_to((np.arange(NE) * CAP).astype(np.float32)[None, :], (128, NE)).copy()
    return dict(ebias=eb.reshape(128, 3072).astype(np.float32), ident=ident, tri=tri, ones=ones, ecol=ecol)


def _bc(v):
    return np.ascontiguousarray(np.broadcast_to(np.asarray(v, np.float32).reshape(1, -1), (128, v.size)))


def make_in_maps(inputs, stage="full"):
    x = np.asarray(inputs["x"], np.float32)
    cst = _consts()
    shared = dict(cst)
    shared["sinks"] = _bc(inputs["attn_sinks"][0])
    shared["ln1g"] = _bc(inputs["ln1_g"][0]); shared["ln1b"] = _bc(inputs["ln1_b"][0])
    shared["ln2g"] = _bc(inputs["ln2_g"][0]); shared["ln2b"] = _bc(inputs["ln2_b"][0])
    shared["pscale"] = np.ascontiguousarray(np.asarray(inputs["pool_scale"][0], np.float32).reshape(4, 128).T)
    shared["brout"] = _bc(inputs["b_router"][0])
    shared["b1T"] = np.ascontiguousarray(
        np.asarray(inputs["b_mlp1"][0], np.float32).reshape(NE, 16, 128).transpose(2, 0, 1).reshape(128, NE * 16))
    shared["b2"] = np.ascontiguousarray(np.asarray(inputs["b_mlp2"][0], np.float32))
    shared["wr"] = np.ascontiguousarray(np.asarray(inputs["w_router"][0], np.float32))
    shared["w_in"] = np.ascontiguousarray(np.asarray(inputs["w_in"][0], np.float32))
    shared["wab"] = np.ascontiguousarray(np.asarray(inputs["w_attn_branch"][0], np.float32))
    shared["wpg"] = np.ascontiguousarray(np.asarray(inputs["w_pool_group"][0], np.float32).reshape(512, 128))
    shared["wpb"] = np.ascontiguousarray(np.asarray(inputs["w_pool_branch"][0], np.float32))
    shared["wout"] = np.ascontiguousarray(np.asarray(inputs["w_out"][0], np.float32))
    shared["w1"] = np.ascontiguousarray(np.asarray(inputs["w_mlp1"][0], np.float32))
    shared["w2"] = np.ascontiguousarray(np.asarray(inputs["w_mlp2"][0], np.float32))
    maps = []
    for c in range(NCORES):
        b = c // 4
        s0 = (c % 4) * TOK
        xs = np.zeros((NTILE, SLAB, D), np.float32)
        kval = np.zeros((NTILE, 128, 6), np.float32)
        icnt = np.zeros((NTILE, 128, 64), np.float32)
        for t in range(NTILE):
            st = s0 + t * TT - 128
            lo, hi = max(st, 0), min(st + SLAB, SEQ)
            xs[t, lo - st:hi - st] = x[b, lo:hi]
            pos = st + np.arange(SLAB)
            v = ((pos >= 0) & (pos < SEQ)).astype(np.float32)
            kval[t] = v.reshape(6, 128).T
            for g, w in enumerate((2, 4, 8, 16)):
                for ed in range(2):
                    tt = s0 + t * TT + (np.arange(8) if ed == 0 else 504 + np.arange(8))
                    lo_ = np.maximum(tt - w // 2, 0)
                    hi_ = np.minimum(tt + w // 2 - 1, SEQ - 1)
                    icnt[t, :, g * 16 + ed * 8:g * 16 + ed * 8 + 8] = (1.0 / (hi_ - lo_ + 1)).astype(np.float32)[None, :]
        m = dict(shared)
        m["xs"] = xs; m["kval"] = kval; m["icnt"] = icnt
        if stage != "full":
            for k in ("ln2g", "ln2b", "b1T", "b2", "w1", "w2"):
                m.pop(k)
        maps.append(m)
    return maps


def kernel(**inputs):
    nc = build_program("full")
    maps = make_in_maps(inputs)
    res = run_bass_kernel_spmd(nc, maps, core_ids=list(range(NCORES)))
    outs = [np.asarray(r["out"], np.float32) for r in res.results]
    full = np.concatenate(outs, axis=0).reshape(2, SEQ, D)
    return full
```
